# Optimizing a Trainium2 kernel written in Bass

```python
import jax, jax.numpy as jnp
from jax import lax
import numpy as np

D_MODEL = 2048
BATCH = 8
SEQ = 4096
DEPTH = 2

N_MIXERS = 2
NORM_EPS = 1e-6
N_ML_LAYERS = (DEPTH + 1) // 2
N_SSD_LAYERS = DEPTH // 2

ML_HEADS = 4
ML_V_DIM = D_MODEL // ML_HEADS
ML_QK_DIM = ML_V_DIM // 2
ML_QK_TOT = ML_HEADS * ML_QK_DIM
ML_V_TOT = ML_HEADS * ML_V_DIM
ML_SPLITS = (ML_QK_TOT, 2 * ML_QK_TOT, 2 * ML_QK_TOT + ML_V_TOT,
             2 * ML_QK_TOT + 2 * ML_V_TOT, 2 * ML_QK_TOT + 2 * ML_V_TOT + ML_HEADS)
ML_IN_DIM = 2 * ML_QK_TOT + 2 * ML_V_TOT + 2 * ML_HEADS
ML_CHUNK = 64

SSM_D_INNER = 2 * D_MODEL
SSM_HEAD_DIM = 64
SSM_HEADS = SSM_D_INNER // SSM_HEAD_DIM
SSM_GROUPS = 8
SSM_HEADS_PER_GROUP = SSM_HEADS // SSM_GROUPS
SSM_STATE = 128
SSM_CONV = 4
SSM_CHUNK = 128
SSM_CONV_DIM = SSM_D_INNER + 2 * SSM_GROUPS * SSM_STATE
SSM_IN_DIM = SSM_D_INNER + SSM_CONV_DIM + SSM_HEADS

MOE_GROUPS = 8
MOE_EXPERTS_PER_GROUP = 8
MOE_EXPERTS = MOE_GROUPS * MOE_EXPERTS_PER_GROUP
MOE_TOP_K = 2
MOE_D_FF = 512
MOE_BLOCK = 128

kernel_name = "hybrid_mlstm_ssd_hmoe_trunk"


def rms_norm(x, w):
    xf = x.astype(jnp.float32)
    y = xf * lax.rsqrt(jnp.mean(xf * xf, axis=-1, keepdims=True) + NORM_EPS)
    return (y * w.astype(jnp.float32)).astype(x.dtype)


def mlstm_mixer(h, w_in, b_i, b_f, norm_w, w_out):
    B_, S_, _ = h.shape
    nc = S_ // ML_CHUNK
    proj = jnp.einsum('bsd,de->bse', h, w_in).astype(jnp.float32)
    q, k, v, o, ig, fg = jnp.split(proj, list(ML_SPLITS), axis=-1)
    q = q * (ML_QK_DIM ** -0.5)
    ig = ig + b_i.astype(jnp.float32)
    lf = jax.nn.log_sigmoid(fg + b_f.astype(jnp.float32))

    def to_chunks(t, dim):
        return t.reshape(B_, nc, ML_CHUNK, ML_HEADS, dim).transpose(1, 0, 3, 2, 4)

    def gate_chunks(t):
        return t.reshape(B_, nc, ML_CHUNK, ML_HEADS).transpose(1, 0, 3, 2)

    qc = to_chunks(q.reshape(B_, S_, ML_HEADS, ML_QK_DIM), ML_QK_DIM)
    kc = to_chunks(k.reshape(B_, S_, ML_HEADS, ML_QK_DIM), ML_QK_DIM)
    vc = to_chunks(v.reshape(B_, S_, ML_HEADS, ML_V_DIM), ML_V_DIM)
    icc = gate_chunks(ig)
    lfc = gate_chunks(lf)
    causal = jnp.tril(jnp.ones((ML_CHUNK, ML_CHUNK), dtype=bool))

    def step(carry, inp):
        C, n, m = carry
        qb, kb, vb, ib, fb = inp
        b = jnp.cumsum(fb, axis=-1)
        logd = b[..., :, None] - b[..., None, :] + ib[..., None, :]
        logd = jnp.where(causal, logd, -jnp.inf)
        inter = b + m[..., None]
        m_t = jnp.maximum(inter, jnp.max(logd, axis=-1))
        s = jnp.einsum('bhtk,bhsk->bhts', qb, kb) * jnp.exp(logd - m_t[..., None])
        sc = jnp.exp(inter - m_t)
        num = jnp.einsum('bhts,bhsv->bhtv', s, vb) + sc[..., None] * jnp.einsum('bhtk,bhkv->bhtv', qb, C)
        den = jnp.sum(s, axis=-1) + sc * jnp.einsum('bhtk,bhk->bht', qb, n)
        hout = num / jnp.maximum(jnp.abs(den), jnp.exp(-m_t))[..., None]
        b_last = b[..., -1]
        g = b_last[..., None] - b + ib
        m_new = jnp.maximum(b_last + m, jnp.max(g, axis=-1))
        ws = jnp.exp(g - m_new[..., None])
        dec = jnp.exp(b_last + m - m_new)
        C_new = dec[..., None, None] * C + jnp.einsum('bhsk,bhsv->bhkv', ws[..., None] * kb, vb)
        n_new = dec[..., None] * n + jnp.einsum('bhs,bhsk->bhk', ws, kb)
        return (C_new, n_new, m_new), hout

    init = (jnp.zeros((B_, ML_HEADS, ML_QK_DIM, ML_V_DIM), jnp.float32),
            jnp.zeros((B_, ML_HEADS, ML_QK_DIM), jnp.float32),
            jnp.zeros((B_, ML_HEADS), jnp.float32))
    _, hs = lax.scan(step, init, (qc, kc, vc, icc, lfc))
    hs = hs.transpose(1, 0, 3, 2, 4).reshape(B_, S_, ML_HEADS, ML_V_DIM)
    hs = hs * lax.rsqrt(jnp.mean(hs * hs, axis=-1, keepdims=True) + NORM_EPS)
    hs = hs * norm_w.astype(jnp.float32).reshape(ML_HEADS, ML_V_DIM)
    hs = hs.reshape(B_, S_, ML_V_TOT) * jax.nn.sigmoid(o)
    return jnp.einsum('bse,ed->bsd', hs.astype(h.dtype), w_out).astype(h.dtype)


def causal_depthwise_conv(u, w, b):
    c = u.shape[-1]
    out = lax.conv_general_dilated(u, w.astype(u.dtype)[:, None, :], window_strides=(1,),
                                   padding=[(SSM_CONV - 1, 0)],
                                   dimension_numbers=('NWC', 'WIO', 'NWC'),
                                   feature_group_count=c)
    return out + b.astype(u.dtype)


def ssd_mixer(h, w_in, conv_w, conv_b, dt_bias, a_log, d_skip, norm_w, w_out):
    B_, S_, _ = h.shape
    nc = S_ // SSM_CHUNK
    G, HG, P, N, L = SSM_GROUPS, SSM_HEADS_PER_GROUP, SSM_HEAD_DIM, SSM_STATE, SSM_CHUNK
    proj = jnp.einsum('bsd,de->bse', h, w_in).astype(jnp.float32)
    z, xbc, dt = jnp.split(proj, [SSM_D_INNER, SSM_D_INNER + SSM_CONV_DIM], axis=-1)
    xbc = jax.nn.silu(causal_depthwise_conv(xbc, conv_w, conv_b))
    xs, bm, cm = jnp.split(xbc, [SSM_D_INNER, SSM_D_INNER + G * N], axis=-1)
    dt = jax.nn.softplus(dt + dt_bias.astype(jnp.float32))
    a = -jnp.exp(a_log.astype(jnp.float32))
    da = dt * a

    xh = xs.reshape(B_, S_, SSM_HEADS, P)
    xc = jnp.moveaxis(xh.reshape(B_, nc, L, G, HG, P), 1, 0)
    dtc = jnp.moveaxis(dt.reshape(B_, nc, L, G, HG), 1, 0)
    dac = jnp.moveaxis(da.reshape(B_, nc, L, G, HG), 1, 0)
    bc = jnp.moveaxis(bm.reshape(B_, nc, L, G, N), 1, 0)
    cc = jnp.moveaxis(cm.reshape(B_, nc, L, G, N), 1, 0)
    causal = jnp.tril(jnp.ones((L, L), dtype=bool))

    def step(state, inp):
        xb, dtb, dab, bb, cb_ = inp
        cum = jnp.cumsum(dab, axis=1)
        cum_t = cum.transpose(0, 2, 3, 1)
        seg = cum_t[..., :, None] - cum_t[..., None, :]
        decay = jnp.exp(jnp.where(causal, seg, -jnp.inf))
        cbm = jnp.einsum('btgn,bsgn->bgts', cb_, bb)
        wts = cbm[:, :, None] * decay * dtb.transpose(0, 2, 3, 1)[..., None, :]
        y_diag = jnp.einsum('bghts,bsghp->btghp', wts, xb)
        y_off = jnp.einsum('btgn,bghpn->btghp', cb_, state) * jnp.exp(cum)[..., None]
        to_end = jnp.exp(cum[:, -1:] - cum) * dtb
        new_state = state * jnp.exp(cum[:, -1])[..., None, None] + \
            jnp.einsum('bsgn,bsghp->bghpn', bb, to_end[..., None] * xb)
        return new_state, y_diag + y_off

    init = jnp.zeros((B_, G, HG, P, N), jnp.float32)
    _, ys = lax.scan(step, init, (xc, dtc, dac, bc, cc))
    y = jnp.moveaxis(ys, 0, 1).reshape(B_, S_, SSM_HEADS, P)
    y = y + d_skip.astype(jnp.float32)[:, None] * xh
    y = y.reshape(B_, S_, SSM_D_INNER) * jax.nn.silu(z)
    yg = y.reshape(B_, S_, G, SSM_D_INNER // G)
    yg = yg * lax.rsqrt(jnp.mean(yg * yg, axis=-1, keepdims=True) + NORM_EPS)
    y = yg.reshape(B_, S_, SSM_D_INNER) * norm_w.astype(jnp.float32)
    return jnp.einsum('bse,ed->bsd', y.astype(h.dtype), w_out).astype(h.dtype)


def hier_moe(h, w_group, b_group, w_expert, b_expert, w_gate, w_up, w_down):
    B_, S_, D = h.shape
    T = B_ * S_
    A = T * MOE_TOP_K
    xt = h.reshape(T, D)
    g_prob = jax.nn.softmax((xt @ w_group + b_group).astype(jnp.float32), axis=-1)
    g_w, g_idx = lax.top_k(g_prob, 1)
    e_logits = (xt @ w_expert + b_expert).astype(jnp.float32).reshape(T, MOE_GROUPS, MOE_EXPERTS_PER_GROUP)
    e_sel = jnp.take_along_axis(e_logits, g_idx[:, :, None], axis=1)[:, 0]
    e_w, e_loc = lax.top_k(jax.nn.softmax(e_sel, axis=-1), MOE_TOP_K)
    e_w = e_w / jnp.sum(e_w, axis=-1, keepdims=True)
    gate = g_w * e_w
    eid = g_idx * MOE_EXPERTS_PER_GROUP + e_loc

    flat_e = eid.reshape(A)
    flat_w = gate.reshape(A)
    flat_tok = jnp.repeat(jnp.arange(T, dtype=jnp.int32), MOE_TOP_K)
    order = jnp.argsort(flat_e)
    se, stok, sw = flat_e[order], flat_tok[order], flat_w[order]
    counts = jnp.bincount(flat_e, length=MOE_EXPERTS)
    starts = jnp.cumsum(counts) - counts
    padded = (counts + MOE_BLOCK - 1) // MOE_BLOCK * MOE_BLOCK
    pends = jnp.cumsum(padded)
    pstarts = pends - padded
    dest = pstarts[se] + jnp.arange(A, dtype=jnp.int32) - starts[se]
    n_rows = A + MOE_EXPERTS * MOE_BLOCK
    n_blocks = n_rows // MOE_BLOCK
    rows_tok = jnp.full((n_rows,), T, jnp.int32).at[dest].set(stok)
    rows_w = jnp.zeros((n_rows,), jnp.float32).at[dest].set(sw)
    blk_e = jnp.minimum(jnp.searchsorted(pends, jnp.arange(n_blocks) * MOE_BLOCK, side='right'),
                        MOE_EXPERTS - 1)
    x_pad = jnp.concatenate([xt, jnp.zeros((1, D), xt.dtype)], axis=0)
    xb = x_pad[rows_tok].reshape(n_blocks, MOE_BLOCK, D)

    def expert_block(args):
        xblk, e = args
        hid = jax.nn.silu(xblk @ w_gate[e]) * (xblk @ w_up[e])
        return hid @ w_down[e]

    yb = lax.map(expert_block, (xb, blk_e)).reshape(n_rows, D)
    yb = yb * rows_w[:, None].astype(yb.dtype)
    out = jax.ops.segment_sum(yb, rows_tok, num_segments=T + 1)[:T]
    return out.reshape(B_, S_, D).astype(h.dtype)


def setup_inputs(seed: int = 0) -> dict:
    key = jax.random.key(seed)
    ks = jax.random.split(key, 26)
    f32 = jnp.float32
    nrm = lambda k, shp, s: jax.random.normal(k, shp, f32) * s
    dt_u = jax.random.uniform(ks[12], (N_SSD_LAYERS, SSM_HEADS), f32)
    dt0 = jnp.exp(dt_u * (np.log(0.1) - np.log(0.001)) + np.log(0.001)).astype(f32)
    return {
        "x": nrm(ks[0], (BATCH, SEQ, D_MODEL), 1.0),
        "norm_mix_w": 1.0 + nrm(ks[1], (DEPTH, D_MODEL), 0.02),
        "norm_ffn_w": 1.0 + nrm(ks[2], (DEPTH, D_MODEL), 0.02),
        "ml_w_in": nrm(ks[3], (N_ML_LAYERS, D_MODEL, ML_IN_DIM), D_MODEL ** -0.5),
        "ml_b_i": nrm(ks[4], (N_ML_LAYERS, ML_HEADS), 0.1),
        "ml_b_f": jnp.linspace(3.0, 6.0, ML_HEADS, dtype=f32)[None] + nrm(ks[5], (N_ML_LAYERS, ML_HEADS), 0.1),
        "ml_norm_w": 1.0 + nrm(ks[6], (N_ML_LAYERS, ML_V_TOT), 0.02),
        "ml_w_out": nrm(ks[7], (N_ML_LAYERS, ML_V_TOT, D_MODEL), ML_V_TOT ** -0.5),
        "ssd_w_in": nrm(ks[8], (N_SSD_LAYERS, D_MODEL, SSM_IN_DIM), D_MODEL ** -0.5),
        "ssd_conv_w": nrm(ks[9], (N_SSD_LAYERS, SSM_CONV, SSM_CONV_DIM), SSM_CONV ** -0.5),
        "ssd_conv_b": nrm(ks[10], (N_SSD_LAYERS, SSM_CONV_DIM), 0.02),
        "ssd_dt_bias": dt0 + jnp.log(-jnp.expm1(-dt0)),
        "ssd_a_log": jnp.log(jax.random.uniform(ks[11], (N_SSD_LAYERS, SSM_HEADS), f32, 1.0, 16.0)),
        "ssd_d": 1.0 + nrm(ks[13], (N_SSD_LAYERS, SSM_HEADS), 0.1),
        "ssd_norm_w": 1.0 + nrm(ks[14], (N_SSD_LAYERS, SSM_D_INNER), 0.02),
        "ssd_w_out": nrm(ks[15], (N_SSD_LAYERS, SSM_D_INNER, D_MODEL), SSM_D_INNER ** -0.5),
        "moe_w_group": nrm(ks[16], (DEPTH, D_MODEL, MOE_GROUPS), D_MODEL ** -0.5),
        "moe_b_group": nrm(ks[17], (DEPTH, MOE_GROUPS), 0.01),
        "moe_w_expert": nrm(ks[18], (DEPTH, D_MODEL, MOE_EXPERTS), D_MODEL ** -0.5),
        "moe_b_expert": nrm(ks[19], (DEPTH, MOE_EXPERTS), 0.01),
        "moe_w_gate": nrm(ks[20], (DEPTH, MOE_EXPERTS, D_MODEL, MOE_D_FF), D_MODEL ** -0.5),
        "moe_w_up": nrm(ks[21], (DEPTH, MOE_EXPERTS, D_MODEL, MOE_D_FF), D_MODEL ** -0.5),
        "moe_w_down": nrm(ks[22], (DEPTH, MOE_EXPERTS, MOE_D_FF, D_MODEL), MOE_D_FF ** -0.5),
        "final_norm_w": 1.0 + nrm(ks[23], (D_MODEL,), 0.02),
    }


def reference(x, norm_mix_w, norm_ffn_w, ml_w_in, ml_b_i, ml_b_f, ml_norm_w, ml_w_out,
              ssd_w_in, ssd_conv_w, ssd_conv_b, ssd_dt_bias, ssd_a_log, ssd_d, ssd_norm_w, ssd_w_out,
              moe_w_group, moe_b_group, moe_w_expert, moe_b_expert, moe_w_gate, moe_w_up, moe_w_down,
              final_norm_w):
    h = x
    for layer in range(DEPTH):
        j = layer // N_MIXERS
        hn = rms_norm(h, norm_mix_w[layer])
        if layer % N_MIXERS == 0:
            mix = mlstm_mixer(hn, ml_w_in[j], ml_b_i[j], ml_b_f[j], ml_norm_w[j], ml_w_out[j])
        else:
            mix = ssd_mixer(hn, ssd_w_in[j], ssd_conv_w[j], ssd_conv_b[j], ssd_dt_bias[j],
                            ssd_a_log[j], ssd_d[j], ssd_norm_w[j], ssd_w_out[j])
        h = h + mix
        h = h + hier_moe(rms_norm(h, norm_ffn_w[layer]), moe_w_group[layer], moe_b_group[layer],
                         moe_w_expert[layer], moe_b_expert[layer], moe_w_gate[layer],
                         moe_w_up[layer], moe_w_down[layer])
    return rms_norm(h, final_norm_w)
```

```python
import numpy as np
from contextlib import ExitStack
import concourse.bass as bass
import concourse.mybir as mybir
from concourse.bass_utils import run_bass_kernel_spmd

F32 = mybir.dt.float32
BF16 = mybir.dt.bfloat16
I32 = mybir.dt.int32
AF = mybir.ActivationFunctionType
ALU = mybir.AluOpType
AX = mybir.AxisListType

ROT = 30000
NDMASEM = 8

D = 2048
S = 4096
KC = 16
TT = 512
NT = S // TT
NE = 64
CAP = 320
RB = [(0, 128), (128, 128), (256, 64)]
NSLOT = NE * CAP
EPS = 1e-6
ML_IN = 6152
SSD_IN = 10304


class Prog:
    def __init__(self, nc):
        self.nc = nc
        self.ops = []
        self.stack = ExitStack()
        self.st = {}
        self.names = {}

    def sb(self, name, shape, dt):
        return self.stack.enter_context(self.nc.sbuf_tensor("s_" + name, list(shape), dt))

    def ps(self, name, shape, dt):
        return self.stack.enter_context(self.nc.psum_tensor(name, list(shape), dt))

    @staticmethod
    def _norm(k):
        return k if isinstance(k, tuple) else (k,)

    def _conf(self, k):
        name = k[0]
        ks = self.names.get(name, ())
        if len(k) == 1:
            return list(ks)
        out = []
        if k in ks:
            out.append(k)
        if (name,) in ks:
            out.append((name,))
        return out

    def op(self, eng, fn, r=(), w=(), dma=False):
        oid = len(self.ops)
        deps = {}
        r = [self._norm(k) for k in r]
        w = [self._norm(k) for k in w]
        for k in r:
            for c in self._conf(k):
                lw = self.st[c][0]
                if lw is not None:
                    deps[lw] = True
        for k in w:
            for c in self._conf(k):
                s = self.st[c]
                if s[0] is not None:
                    deps.setdefault(s[0], False)
                for rd in s[1]:
                    deps.setdefault(rd, False)
        for k in r:
            if k not in self.st:
                self.st[k] = [None, []]
                self.names.setdefault(k[0], set()).add(k)
            self.st[k][1].append(oid)
        for k in w:
            if k not in self.st:
                self.st[k] = [None, []]
                self.names.setdefault(k[0], set()).add(k)
            if len(k) == 1:
                for c in self._conf(k):
                    self.st[c] = [oid, []]
            self.st[k] = [oid, []]
        self.ops.append(dict(eng=eng, fn=fn, deps=deps, dma=dma, bar=False))
        return oid

    def barrier(self):
        self.ops.append(dict(eng=None, fn=None, deps={}, dma=False, bar=True))
        self.st = {}
        self.names = {}

    def emit(self):
        nc = self.nc
        ops = self.ops
        engs = ["tensor", "vector", "scalar", "gpsimd", "sync"]
        cnt_c = {e: 0 for e in engs}
        cnt_d = {e: 0 for e in engs}
        for o in ops:
            if o["bar"]:
                continue
            e = o["eng"]
            if o["dma"]:
                o["n"] = cnt_d[e]
                cnt_d[e] += 1
            else:
                o["n"] = cnt_c[e]
                cnt_c[e] += 1
        sems_c = {}
        for e in engs:
            nrot = (cnt_c[e] + ROT - 1) // ROT
            sems_c[e] = [self.stack.enter_context(nc.semaphore(f"c_{e}_{i}")) for i in range(nrot)]
        sems_d = {}
        for e in engs:
            ns = min(NDMASEM, cnt_d[e])
            sems_d[e] = [self.stack.enter_context(nc.semaphore(f"d_{e}_{i}")) for i in range(ns)]

        def signal(o):
            if o["dma"]:
                return sems_d[o["eng"]][o["n"] % NDMASEM], 16 * (o["n"] // NDMASEM + 1)
            return sems_c[o["eng"]][o["n"] // ROT], o["n"] % ROT + 1

        per_eng = {e: [] for e in engs}
        last_dma = {e: {} for e in engs}
        last_c = {e: None for e in engs}
        pend_bar = {e: [] for e in engs}
        for oid, o in enumerate(ops):
            if o["bar"]:
                ws = []
                for e2 in engs:
                    if last_c[e2] is not None:
                        ws.append(signal(ops[last_c[e2]]))
                    for slot, d in last_dma[e2].items():
                        ws.append(signal(ops[d]))
                for e2 in engs:
                    pend_bar[e2] = list(ws)
                continue
            e = o["eng"]
            waits = list(pend_bar[e])
            pend_bar[e] = []
            for d, raw in o["deps"].items():
                p = ops[d]
                if (not p["dma"]) and (not o["dma"]) and p["eng"] == e:
                    if e == "tensor" or not raw:
                        continue
                waits.append(signal(p))
            if o["dma"]:
                slot = o["n"] % NDMASEM
                if slot in last_dma[e]:
                    waits.append(signal(ops[last_dma[e][slot]]))
                last_dma[e][slot] = oid
            else:
                last_c[e] = oid
            o["waits"] = waits
            per_eng[e].append(o)

        with nc.Block() as block:
            def run(e, eng):
                known = {}
                for o in per_eng[e]:
                    for sem, val in o["waits"]:
                        key = id(sem)
                        if known.get(key, 0) >= val:
                            continue
                        known[key] = val
                        eng.wait_ge(sem, val)
                    ins = o["fn"](eng)
                    sem, val = signal(o)
                    ins.then_inc(sem, 16 if o["dma"] else 1)
                for i, sem in enumerate(sems_d[e]):
                    n = cnt_d[e]
                    k = (n - i + NDMASEM - 1) // NDMASEM
                    if k > 0:
                        eng.wait_ge(sem, 16 * k)

            if per_eng["sync"]:
                @block.sync
                def _(eng):
                    run("sync", eng)
            if per_eng["gpsimd"]:
                @block.gpsimd
                def _(eng):
                    run("gpsimd", eng)
            if per_eng["scalar"]:
                @block.scalar
                def _(eng):
                    run("scalar", eng)
            if per_eng["vector"]:
                @block.vector
                def _(eng):
                    run("vector", eng)
            if per_eng["tensor"]:
                @block.tensor
                def _(eng):
                    run("tensor", eng)
        self.stack.close()


class Arena:
    def __init__(self, tile, nwords):
        self.t = tile
        self.n = nwords
        self.off = 0

    def reset(self):
        self.off = 0

    def alloc(self, shape, dt, parts=None):
        parts = shape[0]
        free = int(np.prod(shape[1:]))
        words = free if dt in (F32, I32) else (free + 1) // 2
        words = (words + 7) // 8 * 8
        assert self.off + words <= self.n, ("arena overflow", self.off, words, self.n)
        ap = self.t[0:parts, self.off:self.off + words]
        self.off += words
        if dt != F32:
            ap = ap.bitcast(dt)
        ap = ap[:, 0:free]
        if len(shape) == 3:
            ap = ap.rearrange("p (a b) -> p a b", b=shape[2])
        elif len(shape) == 4:
            ap = ap.rearrange("p (a b c) -> p a b c", b=shape[2], c=shape[3])
        return ap


class Builder:
    def __init__(self, stop="all", dbg=False):
        self.stop = stop
        self.dbg = dbg
        nc = bass.Bass("TRN2", target_bir_lowering=False)
        self.nc = nc
        self.P = Prog(nc)
        self.din = {}
        self.jobs = []
        self._decl_io()
        self._alloc_static()

    def _in(self, name, shape, dt=F32):
        self.din[name] = self.nc.dram_tensor(name, list(shape), dt, kind="ExternalInput").ap()
        return self.din[name]

    def _decl_io(self):
        nc = self.nc
        self._in("xT", [D, S])
        self._in("nmw", [128, 32])
        self._in("nfw", [128, 32])
        self._in("fnw", [128, 16])
        self._in("ml_w_in", [D, ML_IN])
        self._in("ml_bi", [4, 1])
        self._in("ml_bf", [4, 1])
        self._in("ml_nw_col", [128, 16])
        self._in("ml_w_out", [D, D])
        self._in("ssd_w_in", [D, SSD_IN])
        self._in("conv_w", [128, 48, 4])
        self._in("conv_b", [128, 48])
        self._in("dtb_col", [64, 1])
        self._in("dtb_bc", [128, 64])
        self._in("alog_col", [64, 1])
        self._in("d_bc", [128, 64])
        self._in("ssd_nw_col", [128, 32])
        self._in("ssd_w_out", [4096, D])
        self._in("moe_wr", [2, D, 72])
        self._in("moe_br_bc", [2, 128, 72])
        self._in("moe_wg", [2, NE, D, 512])
        self._in("moe_wu", [2, NE, D, 512])
        self._in("moe_wd", [2, NE, 512, D])
        self._in("c_ident", [128, 128])
        self._in("c_tri", [128, 128])
        self._in("c_tris", [128, 128])
        self._in("c_neg", [128, 128])
        self._in("c_ebase", [128, 64])
        self._in("c_reset", [64, 512])
        self.outT = nc.dram_tensor("outT", [D, S], F32, kind="ExternalOutput").ap()
        kw = dict(kind="ExternalOutput") if self.dbg else {}
        self.hmixT = nc.dram_tensor("hmixT", [D, S], F32, **kw).ap()
        if self.stop.startswith("A1only"):
            self.hcur = nc.dram_tensor("hcur", [D, S], F32, kind="ExternalInput").ap()
        else:
            self.hcur = nc.dram_tensor("hcur", [D, S], F32, **kw).ap()
        self.xslots = nc.dram_tensor("xslots", [NSLOT, D], BF16, **kw).ap()
        self.yslots = nc.dram_tensor("yslots", [NSLOT, D], F32, **kw).ap()
        if self.dbg:
            self.dbg_slots = nc.dram_tensor("dbg_slots", [128, 64], I32, kind="ExternalOutput").ap()
            self.dbg_gates = nc.dram_tensor("dbg_gates", [128, 64], F32, kind="ExternalOutput").ap()
        self.dbg_out = {}

    def dbg_tensor(self, name, shape, dt=F32):
        t = self.nc.dram_tensor(name, list(shape), dt, kind="ExternalOutput").ap()
        self.dbg_out[name] = t
        return t

    def _alloc_static(self):
        P = self.P
        self.psum = [P.ps(f"psb{i}", [128, 512], F32) for i in range(8)]
        self.bank_i = 0
        self.ident_f = P.sb("ident_f", [128, 128], F32)
        self.ident_b = P.sb("ident_b", [128, 128], BF16)
        self.ones_b = P.sb("ones_b", [128, 128], BF16)
        self.ones_f = P.sb("ones_f", [128, 128], F32)
        self.tri_f = P.sb("tri_f", [128, 128], F32)
        self.tris_b = P.sb("tris_b", [128, 128], BF16)
        self.neg_b = P.sb("neg_b", [128, 128], BF16)
        self.ebase = P.sb("ebase", [128, 64], F32)
        self.nmw = P.sb("nmw", [128, 32], F32)
        self.nfw = P.sb("nfw", [128, 32], F32)
        self.fnw = P.sb("fnw", [128, 16], F32)
        self.NWB = 4
        self.wbuf = [P.sb(f"wbuf{i}", [128, 4096], BF16) for i in range(self.NWB)]
        self.wb_i = 0
        self.slots_i = P.sb("slots_i", [128, 32, 2], I32)
        self.gates = P.sb("gates", [128, 32, 2], F32)
        self.carry_bc = P.sb("carry_bc", [128, 64], F32)
        self.wr_f = P.sb("wr_f", [128, 16, 72], F32)
        self.br_bc = P.sb("br_bc", [128, 72], F32)
        AW = 41984
        self.arena_t = P.sb("arena", [128, AW], F32)
        self.A = Arena(self.arena_t, AW)
        d = self.din
        o = P.op
        o("sync", lambda e: e.dma_start(out=self.ident_f[:], in_=d["c_ident"]), w=["ident_f"], dma=True)
        o("gpsimd", lambda e: e.dma_start(out=self.ident_b[:], in_=d["c_ident"]), w=["ident_b"], dma=True)
        o("sync", lambda e: e.dma_start(out=self.tri_f[:], in_=d["c_tri"]), w=["tri_f"], dma=True)
        o("gpsimd", lambda e: e.dma_start(out=self.tris_b[:], in_=d["c_tris"]), w=["tris_b"], dma=True)
        o("gpsimd", lambda e: e.dma_start(out=self.neg_b[:], in_=d["c_neg"]), w=["neg_b"], dma=True)
        o("sync", lambda e: e.dma_start(out=self.ebase[:], in_=d["c_ebase"]), w=["ebase"], dma=True)
        o("sync", lambda e: e.dma_start(out=self.nmw[:], in_=d["nmw"]), w=["nmw"], dma=True)
        o("sync", lambda e: e.dma_start(out=self.nfw[:], in_=d["nfw"]), w=["nfw"], dma=True)
        o("sync", lambda e: e.dma_start(out=self.fnw[:], in_=d["fnw"]), w=["fnw"], dma=True)
        o("vector", lambda e: e.memset(self.ones_b[:], 1.0), w=["ones_b"])
        o("vector", lambda e: e.memset(self.ones_f[:], 1.0), w=["ones_f"])
        self.eps_col = P.sb("eps_col", [128, 1], F32)
        o("vector", lambda e: e.memset(self.eps_col[:], EPS), w=["eps_col"])
        self.static_keys = ["ident_f", "ident_b", "tri_f", "tris_b", "neg_b", "ebase", "nmw", "nfw", "fnw",
                            "ones_b", "ones_f"]

    def bank(self):
        i = self.bank_i
        self.bank_i = (self.bank_i + 1) % 6
        return i

    def pb(self, i):
        return self.psum[i]

    def pbf(self, i):
        return self.psum[i][:].bitcast(BF16)

    def mm(self, out, lhsT, rhs, start, stop, r, w):
        self.P.op("tensor", lambda e: e.matmul(out, lhsT=lhsT, rhs=rhs, start=start, stop=stop), r=r, w=w)

    def tr(self, out, in_, ident, r, w):
        self.P.op("tensor", lambda e: e.transpose(out, in_, ident), r=r, w=w)

    def V(self, fn, r, w):
        self.P.op("vector", fn, r=r, w=w)

    def Sc(self, fn, r, w):
        self.P.op("scalar", fn, r=r, w=w)

    def dma(self, out, in_, r, w, q="sync"):
        self.P.op(q, lambda e: e.dma_start(out=out, in_=in_), r=r, w=w, dma=True)

    def bcreg(self, e):
        if getattr(self, "_bcreg", None) is None:
            self._bcreg = e.alloc_register("bcreg")
            e.reg_mov(self._bcreg, NSLOT - 1)
        return self._bcreg

    def phase_barrier(self):
        self.P.barrier()
        self.A.reset()

    def add_job(self, load, compute):
        self.jobs.append((load, compute))

    def run_jobs(self, extra=()):
        jobs = self.jobs
        self.jobs = []
        lj = [i for i, (l, c) in enumerate(jobs) if l is not None]
        wl = list(self.wbuf) + list(extra)
        depth = len(wl) - 1
        bufs = {}
        nxt = 0
        self.wb_i = 0

        def issue(k):
            i = lj[k]
            b = self.wb_i
            self.wb_i = (self.wb_i + 1) % len(wl)
            bufs[i] = b
            jobs[i][0](wl[b], ("wbuf", b))

        for k in range(min(depth, len(lj))):
            issue(k)
        nxt = min(depth, len(lj))
        for i, (l, c) in enumerate(jobs):
            if l is not None:
                if nxt < len(lj):
                    issue(nxt)
                    nxt += 1
                c(wl[bufs[i]], ("wbuf", bufs[i]))
            else:
                c(None, None)

    def wload(self, W, r0, kc_n, c0, ncols):
        def load(wb, key):
            src = W[r0:r0 + kc_n * 128, c0:c0 + ncols].rearrange("(kc p) e -> p kc e", p=128)
            dst = wb[:, 0:kc_n * ncols].rearrange("p (kc e) -> p kc e", e=ncols)
            self.P.op("gpsimd", lambda e: e.dma_start(out=dst, in_=src), w=[key], dma=True)
        return load

    def norm_in(self, src, j, nw, hnT, rstd_bc, rstd_col, tmp_bc):
        A = self
        bk = 6
        hch = [self.t_hch[i] for i in range(3)]
        for kc in range(KC):
            hb = hch[kc % 3]
            hk = ("hch", kc % 3)
            self.dma(hb, src[kc * 128:(kc + 1) * 128, j * TT:(j + 1) * TT], r=[], w=[hk])
            sq = self.t_sq[kc % 2]
            sk = ("sq", kc % 2)
            self.Sc(lambda e, hb=hb, sq=sq: e.activation(out=sq, in_=hb, func=AF.Square), r=[hk], w=[sk])
            self.mm(self.pb(bk)[:, :], self.ones_b[:, :], sq, kc == 0, kc == KC - 1, r=[sk], w=[("ps", bk)])
            self.V(lambda e, hb=hb, kc=kc: e.tensor_scalar(out=hnT[:, kc, :], in0=hb, scalar1=nw[:, kc:kc + 1],
                                                         scalar2=None, op0=ALU.mult), r=[hk], w=[("hnT", kc)])
        self.rstd_from(self.pb(bk)[:, :], ("ps", bk), rstd_bc, "rstd_bc", tmp_bc, "tmp_bc", 1.0 / D)
        b2 = self.bank()
        for i in range(4):
            self.mm(self.pb(b2)[:, i:i + 1], rstd_bc[0:1, i * 128:(i + 1) * 128], self.ones_f[0:1, 0:1], True, True,
                    r=["rstd_bc"], w=[("ps", b2)])
        self.V(lambda e: e.tensor_copy(out=rstd_col, in_=self.pb(b2)[:, 0:4]), r=[("ps", b2)], w=["rstd_col"])

    def rstd_from(self, ss, ss_key, out, out_key, tmp, tmp_key, inv_n):
        self.Sc(lambda e: e.activation(out=tmp, in_=ss, func=AF.Ln, scale=inv_n, bias=self.eps_col[0:ss.shape[0], 0:1]),
                r=[ss_key], w=[tmp_key])
        self.Sc(lambda e: e.activation(out=out, in_=tmp, func=AF.Exp, scale=-0.5), r=[tmp_key], w=[out_key])

    def alloc_common(self):
        A = self.A
        self.t_hch = [A.alloc([128, TT], F32) for _ in range(3)]
        self.t_sq = [A.alloc([128, TT], BF16) for _ in range(2)]
        self.t_hnT = A.alloc([128, KC, TT], BF16)
        self.t_rstd_bc = A.alloc([128, TT], F32)
        self.t_tmp_bc = A.alloc([128, TT], F32)
        self.t_rstd_col = A.alloc([128, 4], F32)
        self.t_rows = A.alloc([128, 4, D], BF16)
        self.t_hm = [A.alloc([128, TT], F32) for _ in range(2)]
        self.t_xnb = [A.alloc([128, TT], BF16) for _ in range(2)]
        self.t_r = {n: A.alloc([128, w], F32) for n, w in
                    [("ss2", 4), ("rs2", 4), ("lg", 72), ("gmax", 1), ("ngmax", 1), ("gex", 8), ("gsum", 1),
                     ("gw", 1), ("goh", 8), ("t88", 64), ("esel", 8), ("top8", 8), ("oh1", 8), ("oh2", 8),
                     ("dd", 1), ("ex", 1), ("ex1", 1), ("ew1", 1), ("ew2", 1), ("o641", 64), ("o642", 64),
                     ("cnt", 64), ("tt", 64), ("j64", 64), ("pos", 1), ("base", 1), ("ovf", 1), ("nov", 1),
                     ("slf", 1)]}
        self.t_Ab = A.alloc([128, 64], BF16)

    def moe_layer_setup(self, l):
        d = self.din
        self.dma(self.wr_f[:], d["moe_wr"][l].rearrange("(kc p) e -> p kc e", p=128), r=[], w=["wr_f"])
        self.dma(self.br_bc[:], d["moe_br_bc"][l], r=[], w=["br_bc"])
        for kc in range(KC):
            self.V(lambda e, kc=kc: e.tensor_scalar(out=self.wr_f[:, kc, :], in0=self.wr_f[:, kc, :],
                                                   scalar1=self.nfw[:, l * 16 + kc:l * 16 + kc + 1], scalar2=None,
                                                   op0=ALU.mult), r=["wr_f"], w=["wr_f"])
        self.V(lambda e: e.memset(self.carry_bc[:], 0.0), r=[], w=["carry_bc"])

    def defer(self, fn):
        pend = self.__dict__.setdefault("_pend", [])
        if pend:
            pend.pop()()
        pend.append(fn)

    def flush_defer(self):
        pend = self.__dict__.setdefault("_pend", [])
        while pend:
            pend.pop()()

    def outproj_chunk(self, l, j, dc, src, pbank):
        def load(dcx):
            self.dma(self.t_hch[dcx % 3], src[dcx * 128:(dcx + 1) * 128, j * TT:(j + 1) * TT], r=[],
                     w=[("hch", dcx % 3)])
        if dc == 0:
            load(0)
            load(1)
        if dc + 2 < KC:
            load(dc + 2)
        hb = self.t_hch[dc % 3]
        hk = ("hch", dc % 3)
        hm = self.t_hm[dc % 2]
        mk = ("hm", dc % 2)
        self.V(lambda e: e.tensor_tensor(out=hm, in0=self.pb(pbank)[:, :], in1=hb, op=ALU.add),
               r=[("ps", pbank), hk], w=[mk])
        self.dma(self.hmixT[dc * 128:(dc + 1) * 128, j * TT:(j + 1) * TT], hm, r=[mk], w=[("hmixT", j)])
        sq = self.t_sq[dc % 2]
        sk = ("sq", dc % 2)
        self.Sc(lambda e: e.activation(out=sq, in_=hm, func=AF.Square), r=[mk], w=[sk])
        xnb = self.t_xnb[dc % 2]
        xk = ("xnb", dc % 2)
        self.V(lambda e: e.tensor_scalar(out=xnb, in0=hm, scalar1=self.nfw[:, l * 16 + dc:l * 16 + dc + 1],
                                         scalar2=None, op0=ALU.mult), r=[mk], w=[xk])

        def part2():
            for i in range(4):
                self.mm(self.pb(7)[:, 288 + i:289 + i], sq[:, i * 128:(i + 1) * 128], self.ones_b[:, 0:1],
                        dc == 0 and i == 0, dc == KC - 1, r=[sk], w=[("ps", 7)])
                self.mm(self.pb(7)[:, i * 72:(i + 1) * 72], hm[:, i * 128:(i + 1) * 128], self.wr_f[:, dc, :],
                        False, dc == KC - 1, r=[mk, "wr_f"], w=[("ps", 7)])
            b = self.bank()
            pv = self.pbf(b)[:, 0:512].rearrange("p (i c) -> p i c", c=128)
            for i in range(4):
                self.tr(pv[:, i, :], xnb[:, i * 128:(i + 1) * 128], self.ident_b[:, :], r=[xk], w=[("ps", b)])
            self.Sc(lambda e: e.activation(out=self.t_rows[:, :, dc * 128:(dc + 1) * 128], in_=pv, func=AF.Copy),
                    r=[("ps", b)], w=[("rows", dc)])
        self.defer(part2)

    def moe_route(self, l, j):
        T = self.t_r
        V = self.V
        Sc = self.Sc
        self.flush_defer()
        V(lambda e: e.tensor_copy(out=T["ss2"], in_=self.pb(7)[:, 288:292]), r=[("ps", 7)], w=["ss2"])
        self.rstd_from(T["ss2"], "ss2", T["rs2"], "rs2", T["ss2"], "ss2", 1.0 / D)
        for i in range(4):
            V(lambda e, i=i: e.tensor_scalar(out=self.t_rows[:, i, :], in0=self.t_rows[:, i, :],
                                             scalar1=T["rs2"][:, i:i + 1], scalar2=None, op0=ALU.mult),
              r=["rows", "rs2"], w=["rows"])
        for i in range(4):
            ti = j * 4 + i
            V(lambda e, i=i: e.scalar_tensor_tensor(out=T["lg"], in0=self.pb(7)[:, i * 72:(i + 1) * 72],
                                                    scalar=T["rs2"][:, i:i + 1], in1=self.br_bc[:, :],
                                                    op0=ALU.mult, op1=ALU.add),
              r=[("ps", 7), "rs2", "br_bc"], w=["lg"])
            V(lambda e: e.tensor_reduce(out=T["gmax"], in_=T["lg"][:, 0:8], axis=AX.X, op=ALU.max),
              r=["lg"], w=["gmax"])
            V(lambda e: e.tensor_scalar(out=T["ngmax"], in0=T["gmax"], scalar1=-1.0, scalar2=None, op0=ALU.mult),
              r=["gmax"], w=["ngmax"])
            Sc(lambda e: e.activation(out=T["gex"], in_=T["lg"][:, 0:8], func=AF.Exp, bias=T["ngmax"][:, 0:1],
                                      accum_out=T["gsum"]), r=["lg", "ngmax"], w=["gex", "gsum"])
            V(lambda e: e.reciprocal(out=T["gw"], in_=T["gsum"]), r=["gsum"], w=["gw"])
            V(lambda e: e.tensor_scalar(out=T["goh"], in0=T["lg"][:, 0:8], scalar1=T["gmax"][:, 0:1], scalar2=None,
                                        op0=ALU.is_equal), r=["lg", "gmax"], w=["goh"])
            t88 = T["t88"].rearrange("p (j g) -> p j g", g=8)
            V(lambda e: e.tensor_tensor(out=t88, in0=T["lg"][:, 8:72].rearrange("p (g j) -> p j g", g=8),
                                        in1=T["goh"].unsqueeze(1).to_broadcast([128, 8, 8]), op=ALU.mult),
              r=["lg", "goh"], w=["t88"])
            V(lambda e: e.tensor_reduce(out=T["esel"], in_=t88, axis=AX.X, op=ALU.add), r=["t88"], w=["esel"])
            V(lambda e: e.max(out=T["top8"], in_=T["esel"]), r=["esel"], w=["top8"])
            V(lambda e: e.tensor_scalar(out=T["oh1"], in0=T["esel"], scalar1=T["top8"][:, 0:1], scalar2=None,
                                        op0=ALU.is_equal), r=["esel", "top8"], w=["oh1"])
            V(lambda e: e.tensor_scalar(out=T["oh2"], in0=T["esel"], scalar1=T["top8"][:, 1:2], scalar2=None,
                                        op0=ALU.is_equal), r=["esel", "top8"], w=["oh2"])
            V(lambda e: e.tensor_tensor(out=T["dd"], in0=T["top8"][:, 1:2], in1=T["top8"][:, 0:1], op=ALU.subtract),
              r=["top8"], w=["dd"])
            Sc(lambda e: e.activation(out=T["ex"], in_=T["dd"], func=AF.Exp), r=["dd"], w=["ex"])
            V(lambda e: e.tensor_scalar(out=T["ex1"], in0=T["ex"], scalar1=1.0, scalar2=None, op0=ALU.add),
              r=["ex"], w=["ex1"])
            V(lambda e: e.reciprocal(out=T["ew1"], in_=T["ex1"]), r=["ex1"], w=["ew1"])
            V(lambda e: e.tensor_tensor(out=T["ew2"], in0=T["ex"], in1=T["ew1"], op=ALU.mult),
              r=["ex", "ew1"], w=["ew2"])
            for k, (ohn, o64n, ewn) in enumerate([("oh1", "o641", "ew1"), ("oh2", "o642", "ew2")]):
                V(lambda e, ohn=ohn, o64n=o64n: e.tensor_tensor(
                    out=T[o64n].rearrange("p (g j) -> p g j", j=8),
                    in0=T["goh"].unsqueeze(2).to_broadcast([128, 8, 8]),
                    in1=T[ohn].unsqueeze(1).to_broadcast([128, 8, 8]), op=ALU.mult),
                  r=["goh", ohn], w=[o64n])
            V(lambda e: e.tensor_tensor(out=self.t_Ab, in0=T["o641"], in1=T["o642"], op=ALU.add),
              r=["o641", "o642"], w=["Ab"])
            b = self.bank()
            self.mm(self.pb(b)[:, 0:64], self.tris_b[:, :], self.t_Ab, True, True, r=["Ab"], w=[("ps", b)])
            self.mm(self.pb(b)[:, 64:128], self.ones_b[:, :], self.t_Ab, True, True, r=["Ab"], w=[("ps", b)])
            V(lambda e, b=b: e.tensor_tensor(out=T["cnt"], in0=self.pb(b)[:, 0:64], in1=self.carry_bc[:, :], op=ALU.add),
              r=[("ps", b), "carry_bc"], w=["cnt"])
            V(lambda e, b=b: e.tensor_tensor(out=self.carry_bc[:, :], in0=self.pb(b)[:, 64:128], in1=self.carry_bc[:, :],
                                             op=ALU.add), r=[("ps", b), "carry_bc"], w=["carry_bc"])
            for k, (o64n, ewn) in enumerate([("o641", "ew1"), ("o642", "ew2")]):
                V(lambda e, o64n=o64n: e.tensor_tensor(out=T["j64"], in0=T[o64n], in1=T["cnt"], op=ALU.mult),
                  r=[o64n, "cnt"], w=["j64"])
                V(lambda e: e.tensor_reduce(out=T["pos"], in_=T["j64"], axis=AX.X, op=ALU.add), r=["j64"], w=["pos"])
                V(lambda e, o64n=o64n: e.tensor_tensor(out=T["j64"], in0=T[o64n], in1=self.ebase[:, :], op=ALU.mult),
                  r=[o64n, "pos"], w=["j64"])
                V(lambda e: e.tensor_reduce(out=T["base"], in_=T["j64"], axis=AX.X, op=ALU.add), r=["j64"], w=["base"])
                V(lambda e: e.tensor_scalar(out=T["ovf"], in0=T["pos"], scalar1=float(CAP) - 0.5, scalar2=None,
                                            op0=ALU.is_gt), r=["pos"], w=["ovf"])
                V(lambda e: e.tensor_scalar(out=T["nov"], in0=T["ovf"], scalar1=-1.0, scalar2=1.0, op0=ALU.mult,
                                            op1=ALU.add), r=["ovf"], w=["nov"])
                V(lambda e: e.tensor_tensor(out=T["slf"], in0=T["pos"], in1=T["base"], op=ALU.add),
                  r=["pos", "base"], w=["slf"])
                V(lambda e: e.scalar_tensor_tensor(out=T["slf"], in0=T["ovf"], scalar=1.0e6, in1=T["slf"],
                                                   op0=ALU.mult, op1=ALU.add), r=["ovf", "slf"], w=["slf"])
                V(lambda e, k=k, ti=ti: e.tensor_copy(out=self.slots_i[:, ti, k:k + 1], in_=T["slf"]),
                  r=["slf"], w=[("slots", ti)])
                V(lambda e, k=k, ti=ti, ewn=ewn: e.scalar_tensor_tensor(
                    out=self.gates[:, ti, k:k + 1], in0=T[ewn], scalar=T["gw"][:, 0:1], in1=T["nov"],
                    op0=ALU.mult, op1=ALU.mult), r=[ewn, "gw", "nov"], w=[("gates", ti)])
                self.P.op("gpsimd", lambda e, i=i, k=k, ti=ti: e.indirect_dma_start(
                    out=self.xslots[:, :], out_offset=bass.IndirectOffsetOnAxis(ap=self.slots_i[:, ti, k:k + 1], axis=0),
                    in_=self.t_rows[:, i, :], in_offset=None, bounds_check=self.bcreg(e), oob_is_err=False),
                    r=["rows", ("slots", ti)], w=[("xs_sc", ti * 2 + k)], dma=True)

    def phase_A0(self):
        d = self.din
        A = self.A
        P = self.P
        V, Sc = self.V, self.Sc
        self.alloc_common()
        qT = A.alloc([128, 8, TT], BF16)
        kT = A.alloc([128, 8, TT], BF16)
        ktm = A.alloc([128, 4, 1024], BF16)
        vtm = A.alloc([128, 4, 2048], BF16)
        og = A.alloc([128, 4, 2048], BF16)
        hsT = self.t_hnT
        Cf = A.alloc([128, 4, 2, 512], F32)
        Cb = A.alloc([128, 4, 2, 512], BF16)
        nf = A.alloc([128, 8], F32)
        nb = A.alloc([128, 8], BF16)
        nwcol = A.alloc([128, 16], F32)
        wgate = A.alloc([128, KC, 8], BF16)
        junk = A.alloc([128, 512], BF16)
        PT = [A.alloc([128, 128], BF16) for _ in range(3)]
        kw = [A.alloc([128, 256], BF16) for _ in range(3)]
        G = {n: A.alloc([4, w], F32) for n, w in
             [("ig", 512), ("fg", 512), ("F", 512), ("M", 512), ("tw", 512), ("w", 512),
              ("w2", 512), ("z", 512), ("Mprev", 4), ("Mend", 4), ("dec", 4), ("Fc", 1), ("Mc", 1), ("bi", 1),
              ("bf", 1), ("bd", 16)]}
        G["t1"] = G["fg"]
        G["a"] = G["ig"]
        gcols = A.alloc([128, 48], F32)
        decbc = A.alloc([128, 16], F32)
        sm = {n: A.alloc([128, 1], F32) for n in ["ssq", "dn", "rden", "t", "rs", "scale"]}
        ones4 = A.alloc([4, 512], F32)
        zeros4 = A.alloc([4, 512], F32)
        V(lambda e: e.memset(ones4, 1.0), r=[], w=["ones4"])
        V(lambda e: e.memset(zeros4, 0.0), r=[], w=["zeros4"])
        self.t_ones4 = ones4
        self.arena_used_A0 = A.off

        self.dma(nwcol, d["ml_nw_col"], r=[], w=["nwcol"])
        P.op("gpsimd", lambda e: e.dma_start(
            out=wgate, in_=d["ml_w_in"][:, 6144:6152].rearrange("(kc p) e -> p kc e", p=128)), w=["wgate"], dma=True)
        self.dma(G["bi"], d["ml_bi"], r=[], w=["bi"])
        self.dma(G["bf"], d["ml_bf"], r=[], w=["bf"])
        V(lambda e: e.memset(Cf, 0.0), r=[], w=["Cf"])
        V(lambda e: e.memset(Cb, 0.0), r=[], w=["Cb"])
        V(lambda e: e.memset(nf, 0.0), r=[], w=["nf"])
        V(lambda e: e.memset(nb, 0.0), r=[], w=["nb"])
        V(lambda e: e.memset(G["Fc"], 0.0), r=[], w=["Fc"])
        V(lambda e: e.memset(G["Mc"], 0.0), r=[], w=["Mc"])
        self.moe_layer_setup(0)
        hnT, rstd_bc, rstd_col = self.t_hnT, self.t_rstd_bc, self.t_rstd_col
        Wi = d["ml_w_in"]
        ntiles = NT if self.stop not in ("A0t1",) else 1

        for j in range(ntiles):
            self.add_job(None, lambda wb, key, j=j: self.norm_in(d["xT"], j, self.nmw[:, 0:16], hnT, rstd_bc,
                                                                rstd_col, self.t_tmp_bc))
            for blk in range(8):
                def comp(wb, key, blk=blk):
                    wv = wb[:, 0:KC * 256].rearrange("p (kc e) -> p kc e", e=256)
                    for ec in range(2):
                        ch = blk * 2 + ec
                        b = self.bank()
                        for kc in range(KC):
                            self.mm(self.pb(b)[:, :], wv[:, kc, ec * 128:(ec + 1) * 128], hnT[:, kc, :], kc == 0,
                                    kc == KC - 1, r=[key, "hnT"], w=[("ps", b)])
                        if ch < 8:
                            V(lambda e, b=b, ch=ch: e.scalar_tensor_tensor(
                                out=qT[:, ch, :], in0=self.pb(b)[:, :], scalar=1.0 / 16.0, in1=rstd_bc,
                                op0=ALU.mult, op1=ALU.mult), r=[("ps", b), "rstd_bc"], w=["qT"])
                        else:
                            V(lambda e, b=b, ch=ch: e.tensor_tensor(out=kT[:, ch - 8, :], in0=self.pb(b)[:, :],
                                                                    in1=rstd_bc, op=ALU.mult),
                              r=[("ps", b), "rstd_bc"], w=["kT"])
                self.add_job(self.wload(Wi, 0, KC, blk * 256, 256), comp)
            for blk in range(20):
                def comp(wb, key, blk=blk):
                    wv = wb[:, 0:KC * 256].rearrange("p (kc e) -> p kc e", e=256)
                    for i in range(4):
                        b = self.bank()
                        for kc in range(KC):
                            self.mm(self.pb(b)[:, 0:256], hnT[:, kc, i * 128:(i + 1) * 128], wv[:, kc, :], kc == 0,
                                    kc == KC - 1, r=[key, "hnT"], w=[("ps", b)])
                        if blk < 4:
                            Sc(lambda e, b=b, i=i: e.activation(out=ktm[:, i, blk * 256:(blk + 1) * 256],
                                                                in_=self.pb(b)[:, 0:256], func=AF.Copy,
                                                                scale=rstd_col[:, i:i + 1]),
                               r=[("ps", b), "rstd_col"], w=["ktm"])
                        elif blk < 12:
                            c0 = (blk - 4) * 256
                            Sc(lambda e, b=b, i=i, c0=c0: e.activation(out=vtm[:, i, c0:c0 + 256],
                                                                       in_=self.pb(b)[:, 0:256], func=AF.Copy,
                                                                       scale=rstd_col[:, i:i + 1]),
                               r=[("ps", b), "rstd_col"], w=["vtm"])
                        else:
                            c0 = (blk - 12) * 256
                            Sc(lambda e, b=b, i=i, c0=c0: e.activation(out=og[:, i, c0:c0 + 256],
                                                                       in_=self.pb(b)[:, 0:256], func=AF.Sigmoid,
                                                                       scale=rstd_col[:, i:i + 1]),
                               r=[("ps", b), "rstd_col"], w=["og"])
                self.add_job(self.wload(Wi, 0, KC, 1024 + blk * 256, 256), comp)

            def rec(wb, key, j=j):
                bi_, bf_ = self.bank(), self.bank()
                for kc in range(KC):
                    self.mm(self.pb(bi_)[0:4, :], wgate[:, kc, 0:4], hnT[:, kc, :], kc == 0, kc == KC - 1,
                            r=["wgate", "hnT"], w=[("ps", bi_)])
                for kc in range(KC):
                    self.mm(self.pb(bf_)[0:4, :], wgate[:, kc, 4:8], hnT[:, kc, :], kc == 0, kc == KC - 1,
                            r=["wgate", "hnT"], w=[("ps", bf_)])
                V(lambda e: e.tensor_tensor(out=G["ig"], in0=self.pb(bi_)[0:4, :], in1=rstd_bc[0:4, :], op=ALU.mult),
                  r=[("ps", bi_), "rstd_bc"], w=["ig"])
                V(lambda e: e.tensor_scalar(out=G["ig"], in0=G["ig"], scalar1=G["bi"][:, 0:1], scalar2=None,
                                            op0=ALU.add), r=["ig", "bi"], w=["ig"])
                V(lambda e: e.tensor_tensor(out=G["fg"], in0=self.pb(bf_)[0:4, :], in1=rstd_bc[0:4, :], op=ALU.mult),
                  r=[("ps", bf_), "rstd_bc"], w=["fg"])
                V(lambda e: e.tensor_scalar(out=G["fg"], in0=G["fg"], scalar1=G["bf"][:, 0:1], scalar2=None,
                                            op0=ALU.add), r=["fg", "bf"], w=["fg"])
                Sc(lambda e: e.activation(out=G["t1"], in_=G["fg"], func=AF.Exp, scale=-1.0), r=["fg"], w=["fg"])
                Sc(lambda e: e.activation(out=G["t1"], in_=G["t1"], func=AF.Ln, bias=1.0), r=["fg"], w=["fg"])
                V(lambda e: e.tensor_tensor_scan(out=G["F"], data0=self.t_ones4, data1=G["t1"],
                                                 initial=G["Fc"][:, 0:1], op0=ALU.mult, op1=ALU.subtract),
                  r=["fg", "Fc", "ones4"], w=["F"])
                V(lambda e: e.tensor_copy(out=G["Fc"], in_=G["F"][:, 511:512]), r=["F"], w=["Fc"])
                V(lambda e: e.tensor_tensor(out=G["a"], in0=G["ig"], in1=G["F"], op=ALU.subtract),
                  r=["ig", "F"], w=["ig"])
                V(lambda e: e.tensor_tensor_scan(out=G["M"], data0=zeros4, data1=G["a"],
                                                 initial=G["Mc"][:, 0:1], op0=ALU.add, op1=ALU.max),
                  r=["ig", "Mc", "zeros4"], w=["M"])
                V(lambda e: e.tensor_copy(out=G["Mprev"][:, 0:1], in_=G["Mc"]), r=["Mc"], w=["Mprev"])
                V(lambda e: e.tensor_copy(out=G["Mprev"][:, 1:4], in_=G["M"][:, 127:384:128]), r=["M"], w=["Mprev"])
                V(lambda e: e.tensor_copy(out=G["Mend"], in_=G["M"][:, 127:512:128]), r=["M"], w=["Mend"])
                V(lambda e: e.tensor_copy(out=G["Mc"], in_=G["M"][:, 511:512]), r=["M"], w=["Mc"])
                v3 = lambda ap: ap.rearrange("p (c t) -> p c t", t=128)
                bc = lambda ap: ap.unsqueeze(2).to_broadcast([4, 4, 128])
                V(lambda e: e.tensor_tensor(out=v3(G["tw"]), in0=v3(G["a"]), in1=bc(G["Mprev"]), op=ALU.subtract),
                  r=["ig", "Mprev"], w=["tw"])
                Sc(lambda e: e.activation(out=G["w"], in_=G["tw"], func=AF.Exp), r=["tw"], w=["w"])
                V(lambda e: e.tensor_tensor(out=v3(G["tw"]), in0=v3(G["a"]), in1=bc(G["Mend"]), op=ALU.subtract),
                  r=["ig", "Mend", "w"], w=["tw"])
                Sc(lambda e: e.activation(out=G["w2"], in_=G["tw"], func=AF.Exp), r=["tw"], w=["w2"])
                V(lambda e: e.tensor_tensor(out=v3(G["tw"]), in0=v3(G["F"]), in1=bc(G["Mprev"]), op=ALU.add),
                  r=["F", "Mprev", "w2"], w=["tw"])
                Sc(lambda e: e.activation(out=G["z"], in_=G["tw"], func=AF.Exp, scale=-1.0), r=["tw"], w=["z"])
                V(lambda e: e.tensor_tensor(out=G["dec"], in0=G["Mprev"], in1=G["Mend"], op=ALU.subtract),
                  r=["Mprev", "Mend"], w=["dec"])
                Sc(lambda e: e.activation(out=G["dec"], in_=G["dec"], func=AF.Exp), r=["dec"], w=["dec"])
                bt = self.bank()
                for si, sn in enumerate(["w", "w2", "z"]):
                    for c in range(4):
                        o0 = (si * 4 + c) * 4
                        self.tr(self.pb(bt)[:, o0:o0 + 4], G[sn][0:4, c * 128:(c + 1) * 128], self.ident_f[0:4, 0:4],
                                r=[sn], w=[("ps", bt)])
                V(lambda e: e.tensor_copy(out=gcols, in_=self.pb(bt)[:, 0:48]), r=[("ps", bt)], w=["gcols"])
                V(lambda e: e.tensor_tensor(out=G["bd"].rearrange("p (h c) -> p h c", c=4),
                                            in0=self.ident_f[0:4, 0:4].unsqueeze(2).to_broadcast([4, 4, 4]),
                                            in1=G["dec"].unsqueeze(1).to_broadcast([4, 4, 4]), op=ALU.mult),
                  r=["dec"], w=["bd"])
                bd_ = self.bank()
                self.mm(self.pb(bd_)[:, 0:16], self.ones_f[0:4, :], G["bd"], True, True, r=["bd"], w=[("ps", bd_)])
                V(lambda e: e.tensor_copy(out=decbc, in_=self.pb(bd_)[:, 0:16]), r=[("ps", bd_)], w=["decbc"])

                def stA(c, h):
                    cs = slice(c * 128, (c + 1) * 128)
                    wcol = gcols[:, (0 * 4 + c) * 4 + h:(0 * 4 + c) * 4 + h + 1]
                    w2col = gcols[:, (1 * 4 + c) * 4 + h:(1 * 4 + c) * 4 + h + 1]
                    zcol = gcols[:, (2 * 4 + c) * 4 + h:(2 * 4 + c) * 4 + h + 1]
                    dcol = decbc[:, h * 4 + c:h * 4 + c + 1]
                    pi = (c * 4 + h) % 3
                    bs = self.bank()
                    for kk in range(2):
                        self.mm(self.pb(bs)[:, 0:128], kT[:, h * 2 + kk, cs], qT[:, h * 2 + kk, cs], kk == 0, kk == 1,
                                r=["kT", "qT"], w=[("ps", bs)])
                    V(lambda e, bs=bs, pi=pi, wcol=wcol: e.scalar_tensor_tensor(
                        out=PT[pi], in0=self.pb(bs)[:, 0:128], scalar=wcol, in1=self.tri_f[:, :],
                        op0=ALU.mult, op1=ALU.mult), r=[("ps", bs), "gcols"], w=[("PT", pi)])
                    V(lambda e, pi=pi, w2col=w2col, c=c, h=h: e.tensor_scalar(
                        out=kw[pi], in0=ktm[:, c, h * 256:(h + 1) * 256], scalar1=w2col, scalar2=None,
                        op0=ALU.mult), r=["ktm", "gcols"], w=[("kw", pi)])
                def stD(c, h):
                    cs = slice(c * 128, (c + 1) * 128)
                    wcol = gcols[:, (0 * 4 + c) * 4 + h:(0 * 4 + c) * 4 + h + 1]
                    w2col = gcols[:, (1 * 4 + c) * 4 + h:(1 * 4 + c) * 4 + h + 1]
                    zcol = gcols[:, (2 * 4 + c) * 4 + h:(2 * 4 + c) * 4 + h + 1]
                    dcol = decbc[:, h * 4 + c:h * 4 + c + 1]
                    pi = (c * 4 + h) % 3
                    bn = self.bank()
                    self.mm(self.pb(bn)[:, :], PT[pi], vtm[:, c, h * 512:(h + 1) * 512], True, False,
                            r=[("PT", pi), "vtm"], w=[("ps", bn)])
                    for kk in range(2):
                        self.mm(self.pb(bn)[:, :], qT[:, h * 2 + kk, cs], Cb[:, h, kk, :], False, kk == 1,
                                r=["qT", ("Cb", h)], w=[("ps", bn)])
                    bdn = self.bank()
                    self.mm(self.pb(bdn)[:, 0:1], PT[pi], self.ones_b[:, 0:1], True, False,
                            r=[("PT", pi)], w=[("ps", bdn)])
                    for kk in range(2):
                        self.mm(self.pb(bdn)[:, 0:1], qT[:, h * 2 + kk, cs], nb[:, h * 2 + kk:h * 2 + kk + 1],
                                False, kk == 1, r=["qT", ("nb", h)], w=[("ps", bdn)])
                    Sc(lambda e, bn=bn: e.activation(out=junk, in_=self.pb(bn)[:, :], func=AF.Square,
                                                     accum_out=sm["ssq"]), r=[("ps", bn)], w=["junk", "ssq"])
                    Sc(lambda e, bdn=bdn: e.activation(out=sm["dn"], in_=self.pb(bdn)[:, 0:1], func=AF.Abs),
                       r=[("ps", bdn)], w=["dn"])
                    V(lambda e, zcol=zcol: e.tensor_tensor(out=sm["dn"], in0=sm["dn"], in1=zcol, op=ALU.max),
                      r=["dn", "gcols"], w=["dn"])
                    V(lambda e: e.reciprocal(out=sm["rden"], in_=sm["dn"]), r=["dn"], w=["rden"])
                    V(lambda e: e.tensor_tensor(out=sm["t"], in0=sm["rden"], in1=sm["rden"], op=ALU.mult),
                      r=["rden"], w=["t"])
                    V(lambda e: e.tensor_tensor(out=sm["t"], in0=sm["t"], in1=sm["ssq"], op=ALU.mult),
                      r=["t", "ssq"], w=["t"])
                    self.rstd_from(sm["t"], "t", sm["rs"], "rs", sm["t"], "t", 1.0 / 512.0)
                    V(lambda e: e.tensor_tensor(out=sm["scale"], in0=sm["rs"], in1=sm["rden"], op=ALU.mult),
                      r=["rs", "rden"], w=["scale"])
                    V(lambda e, bn=bn, c=c, h=h: e.scalar_tensor_tensor(
                        out=og[:, c, h * 512:(h + 1) * 512], in0=self.pb(bn)[:, :], scalar=sm["scale"][:, 0:1],
                        in1=og[:, c, h * 512:(h + 1) * 512], op0=ALU.mult, op1=ALU.mult),
                      r=[("ps", bn), "scale", "og"], w=["og"])
                    bnn = self.bank()
                    for kk in range(2):
                        bc_ = self.bank()
                        self.mm(self.pb(bc_)[:, :], kw[pi][:, kk * 128:(kk + 1) * 128],
                                vtm[:, c, h * 512:(h + 1) * 512], True, True, r=[("kw", pi), "vtm"],
                                w=[("ps", bc_)])
                        self.mm(self.pb(bnn)[:, kk:kk + 1], kw[pi][:, kk * 128:(kk + 1) * 128],
                                self.ones_b[:, 0:1], True, True, r=[("kw", pi)], w=[("ps", bnn)])
                        V(lambda e, bc_=bc_, h=h, kk=kk, dcol=dcol: e.scalar_tensor_tensor(
                            out=Cf[:, h, kk, :], in0=Cf[:, h, kk, :], scalar=dcol, in1=self.pb(bc_)[:, :],
                            op0=ALU.mult, op1=ALU.add), r=[("ps", bc_), ("Cf", h), "decbc"], w=[("Cf", h)])
                        Sc(lambda e, h=h, kk=kk: e.activation(out=Cb[:, h, kk, :], in_=Cf[:, h, kk, :],
                                                              func=AF.Copy), r=[("Cf", h)], w=[("Cb", h)])
                    V(lambda e, bnn=bnn, h=h, dcol=dcol: e.scalar_tensor_tensor(
                        out=nf[:, h * 2:h * 2 + 2], in0=nf[:, h * 2:h * 2 + 2], scalar=dcol,
                        in1=self.pb(bnn)[:, 0:2], op0=ALU.mult, op1=ALU.add),
                      r=[("ps", bnn), ("nf", h), "decbc"], w=[("nf", h)])
                    V(lambda e, h=h: e.tensor_copy(out=nb[:, h * 2:h * 2 + 2], in_=nf[:, h * 2:h * 2 + 2]),
                      r=[("nf", h)], w=[("nb", h)])
                stp = [(c, h) for c in range(4) for h in range(4)]
                LA0 = 2
                for s_ in range(16 + LA0):
                    if s_ < 16:
                        stA(*stp[s_])
                    if s_ - LA0 >= 0:
                        stD(*stp[s_ - LA0])
                for ec in range(KC):
                    b = self.bank()
                    pv = self.pbf(b)[:, 0:512]
                    for i in range(4):
                        self.tr(pv[:, i * 128:(i + 1) * 128], og[:, i, ec * 128:(ec + 1) * 128], self.ident_b[:, :],
                                r=["og"], w=[("ps", b)])
                    Sc(lambda e, ec=ec, pv=pv: e.activation(out=hsT[:, ec, :], in_=pv, func=AF.Copy,
                                                            scale=nwcol[:, ec:ec + 1]),
                       r=[("ps", b), "nwcol"], w=["hnT"])
            self.add_job(None, rec)

            for blk in range(8):
                def comp(wb, key, blk=blk, j=j):
                    wv = wb[:, 0:KC * 256].rearrange("p (kc e) -> p kc e", e=256)
                    for ec in range(2):
                        dc = blk * 2 + ec
                        b = self.bank()
                        for kc in range(KC):
                            self.mm(self.pb(b)[:, :], wv[:, kc, ec * 128:(ec + 1) * 128], hsT[:, kc, :], kc == 0,
                                    kc == KC - 1, r=[key, "hnT"], w=[("ps", b)])
                        self.outproj_chunk(0, j, dc, d["xT"], b)
                    if blk == 7:
                        self.moe_route(0, j)
                self.add_job(self.wload(d["ml_w_out"], 0, KC, blk * 256, 256), comp)
        self.run_jobs()


    def phase_A1(self):
        d = self.din
        A = self.A
        P = self.P
        V, Sc = self.V, self.Sc
        self.alloc_common()
        hnT, rstd_bc, rstd_col = self.t_hnT, self.t_rstd_bc, self.t_rstd_col
        ynT = A.alloc([128, 32, TT], BF16)
        stf = A.alloc([128, 8, 512], F32)
        stb = A.alloc([128, 512], BF16)
        zs = A.alloc([128, 4, 512], BF16)
        xs = A.alloc([128, 4, 512], BF16)
        Btm = A.alloc([128, 4, 128], BF16)
        BT = A.alloc([128, TT], BF16)
        CT = A.alloc([128, TT], BF16)
        ccar = A.alloc([128, 48, 3], F32)
        cw = A.alloc([128, 48, 4], F32)
        cb = A.alloc([128, 48], F32)
        uext = [A.alloc([128, 515], F32) for _ in range(2)]
        accb = [A.alloc([128, TT], F32) for _ in range(2)]
        xsTc = [A.alloc([128, TT], BF16) for _ in range(3)]
        wdt = A.alloc([128, KC, 64], BF16)
        dtT = A.alloc([64, TT], F32)
        daT = A.alloc([64, TT], F32)
        cumT = A.alloc([64, TT], F32)
        rstm = A.alloc([64, TT], F32)
        alogc = A.alloc([64, 1], F32)
        acol = A.alloc([64, 1], F32)
        dtbc = A.alloc([64, 1], F32)
        rhsd = [A.alloc([64, 64], F32) for _ in range(2)]
        Tm = {n: A.alloc([128, 4, 64], F32) for n in ["dt", "cum", "ncum", "ecum", "cl", "ecl", "te", "tmp"]}
        dtb_bc = A.alloc([128, 64], F32)
        d_bc = A.alloc([128, 64], F32)
        nwcol = A.alloc([128, 32], F32)
        cbmm = [A.alloc([128, 128], F32) for _ in range(2)]
        dec = [A.alloc([128, 128], F32) for _ in range(8)]
        WT = [A.alloc([128, 128], BF16) for _ in range(8)]
        y1 = [A.alloc([128, 512], F32) for _ in range(2)]
        y2 = A.alloc([128, 512], F32)
        junk = A.alloc([128, 512], BF16)
        xw = [A.alloc([128, 512], BF16) for _ in range(2)]
        sm = {n: A.alloc([128, 1], F32) for n in ["ssq", "rs"]}
        Wi = d["ssd_w_in"]
        self.arena_used_A1 = A.off

        self.dma(cw, d["conv_w"], r=[], w=["cw"])
        self.dma(cb, d["conv_b"], r=[], w=["cb"])
        self.dma(rstm, d["c_reset"], r=[], w=["rstm"])
        self.dma(alogc, d["alog_col"], r=[], w=["alogc"])
        self.dma(dtbc, d["dtb_col"], r=[], w=["dtbc"])
        self.dma(dtb_bc, d["dtb_bc"], r=[], w=["dtb_bc"])
        self.dma(d_bc, d["d_bc"], r=[], w=["d_bc"])
        self.dma(nwcol, d["ssd_nw_col"], r=[], w=["nwcol"])
        P.op("gpsimd", lambda e: e.dma_start(
            out=wdt, in_=Wi[:, 10240:10304].rearrange("(kc p) e -> p kc e", p=128)), w=["wdt"], dma=True)
        Sc(lambda e: e.activation(out=acol, in_=alogc, func=AF.Exp), r=["alogc"], w=["acol"])
        V(lambda e: e.tensor_scalar(out=acol, in0=acol, scalar1=-1.0, scalar2=None, op0=ALU.mult), r=["acol"], w=["acol"])
        V(lambda e: e.memset(ccar, 0.0), r=[], w=["ccar"])
        V(lambda e: e.memset(stf, 0.0), r=[], w=["stf"])
        self.moe_layer_setup(1)
        ntiles = NT if self.stop not in ("A1t1", "A1only_t1") else 1
        hv = lambda ap: ap.rearrange("p (h q) -> p h q", q=64)

        def conv_chunk(q, b, out_bf, okey):
            u = uext[q % 2]
            uk = ("uext", q % 2)
            ac = accb[q % 2]
            ak = ("accb", q % 2)
            V(lambda e: e.tensor_tensor(out=u[:, 3:515], in0=self.pb(b)[:, :], in1=rstd_bc, op=ALU.mult),
              r=[("ps", b), "rstd_bc"], w=[uk])
            Sc(lambda e: e.activation(out=u[:, 0:3], in_=ccar[:, q, :], func=AF.Copy), r=[("ccar", q)], w=[uk])
            V(lambda e: e.tensor_scalar(out=ac, in0=u[:, 3:515], scalar1=cw[:, q, 3:4], scalar2=cb[:, q:q + 1],
                                        op0=ALU.mult, op1=ALU.add), r=[uk, "cw", "cb"], w=[ak])
            for k in (2, 1, 0):
                V(lambda e, k=k: e.scalar_tensor_tensor(out=ac, in0=u[:, k:k + 512], scalar=cw[:, q, k:k + 1], in1=ac,
                                                        op0=ALU.mult, op1=ALU.add), r=[uk, ak, "cw"], w=[ak])
            Sc(lambda e: e.activation(out=ccar[:, q, :], in_=u[:, 512:515], func=AF.Copy), r=[uk], w=[("ccar", q)])
            Sc(lambda e: e.activation(out=out_bf, in_=ac, func=AF.Silu), r=[ak], w=[okey])

        def softplus_inplace(t, key):
            Sc(lambda e: e.activation(out=t, in_=t, func=AF.Exp), r=[key], w=[key])
            Sc(lambda e: e.activation(out=t, in_=t, func=AF.Ln, bias=1.0), r=[key], w=[key])

        pend = []

        def defer(fn):
            if len(pend) >= 2:
                pend.pop(0)()
            pend.append(fn)

        def flush():
            while pend:
                pend.pop(0)()

        for j in range(ntiles):
            def pre(wb, key, j=j):
                self.norm_in(self.hcur, j, self.nmw[:, 16:32], hnT, rstd_bc, rstd_col, self.t_tmp_bc)
                b = self.bank()
                for kc in range(KC):
                    self.mm(self.pb(b)[0:64, :], wdt[:, kc, :], hnT[:, kc, :], kc == 0, kc == KC - 1,
                            r=["wdt", "hnT"], w=[("ps", b)])
                V(lambda e: e.tensor_tensor(out=dtT, in0=self.pb(b)[0:64, :], in1=rstd_bc[0:64, :], op=ALU.mult),
                  r=[("ps", b), "rstd_bc"], w=["dtT"])
                V(lambda e: e.tensor_scalar(out=dtT, in0=dtT, scalar1=dtbc[:, 0:1], scalar2=None, op0=ALU.add),
                  r=["dtT", "dtbc"], w=["dtT"])
                softplus_inplace(dtT, "dtT")
                V(lambda e: e.tensor_scalar(out=daT, in0=dtT, scalar1=acol[:, 0:1], scalar2=None, op0=ALU.mult),
                  r=["dtT", "acol"], w=["daT"])
                V(lambda e: e.tensor_tensor_scan(out=cumT, data0=rstm, data1=daT, initial=0.0, op0=ALU.mult,
                                                 op1=ALU.add), r=["daT", "rstm"], w=["cumT"])
                for i in range(4):
                    b2 = self.bank()
                    for kc in range(KC):
                        self.mm(self.pb(b2)[:, 0:64], hnT[:, kc, i * 128:(i + 1) * 128], wdt[:, kc, :], kc == 0,
                                kc == KC - 1, r=["wdt", "hnT"], w=[("ps", b2)])
                    V(lambda e, i=i, b2=b2: e.scalar_tensor_tensor(out=Tm["dt"][:, i, :], in0=self.pb(b2)[:, 0:64],
                                                                   scalar=rstd_col[:, i:i + 1], in1=dtb_bc,
                                                                   op0=ALU.mult, op1=ALU.add),
                      r=[("ps", b2), "rstd_col", "dtb_bc"], w=["Tdt"])
                softplus_inplace(Tm["dt"], "Tdt")
                b3 = self.bank()
                for c in range(4):
                    self.tr(self.pb(b3)[:, c * 64:(c + 1) * 64], cumT[0:64, c * 128:(c + 1) * 128],
                            self.ident_f[0:64, 0:64], r=["cumT"], w=[("ps", b3)])
                V(lambda e: e.tensor_copy(out=Tm["cum"], in_=self.pb(b3)[:, 0:256].rearrange("p (c h) -> p c h", h=64)),
                  r=[("ps", b3)], w=["Tcum"])
                V(lambda e: e.tensor_scalar(out=Tm["ncum"], in0=Tm["cum"], scalar1=-1.0, scalar2=None, op0=ALU.mult),
                  r=["Tcum"], w=["Tncum"])
                Sc(lambda e: e.activation(out=Tm["ecum"], in_=Tm["cum"], func=AF.Exp), r=["Tcum"], w=["Tecum"])
                b4 = self.bank()
                for c in range(4):
                    rd = rhsd[c % 2]
                    rk = ("rhsd", c % 2)
                    V(lambda e, c=c, rd=rd: e.tensor_scalar(out=rd, in0=self.ident_f[0:64, 0:64],
                                                            scalar1=cumT[:, c * 128 + 127:c * 128 + 128], scalar2=None,
                                                            op0=ALU.mult), r=["cumT"], w=[rk])
                    self.mm(self.pb(b4)[:, c * 64:(c + 1) * 64], self.ones_f[0:64, :], rd, True, True, r=[rk],
                            w=[("ps", b4)])
                V(lambda e: e.tensor_copy(out=Tm["cl"], in_=self.pb(b4)[:, 0:256].rearrange("p (c h) -> p c h", h=64)),
                  r=[("ps", b4)], w=["Tcl"])
                Sc(lambda e: e.activation(out=Tm["ecl"], in_=Tm["cl"], func=AF.Exp), r=["Tcl"], w=["Tecl"])
                V(lambda e: e.tensor_tensor(out=Tm["tmp"], in0=Tm["cl"], in1=Tm["cum"], op=ALU.subtract),
                  r=["Tcl", "Tcum"], w=["Ttmp"])
                Sc(lambda e: e.activation(out=Tm["tmp"], in_=Tm["tmp"], func=AF.Exp), r=["Ttmp"], w=["Ttmp"])
                V(lambda e: e.tensor_tensor(out=Tm["te"], in0=Tm["tmp"], in1=Tm["dt"], op=ALU.mult),
                  r=["Ttmp", "Tdt"], w=["Tte"])
            self.add_job(None, pre)

            for g in range(8):
                for blk in range(2):
                    def comp(wb, key, blk=blk, g=g):
                        wv = wb[:, 0:KC * 256].rearrange("p (kc e) -> p kc e", e=256)
                        for i in range(4):
                            b = self.bank()
                            for kc in range(KC):
                                self.mm(self.pb(b)[:, 0:256], hnT[:, kc, i * 128:(i + 1) * 128], wv[:, kc, :], kc == 0,
                                        kc == KC - 1, r=[key, "hnT"], w=[("ps", b)])
                            Sc(lambda e, b=b, i=i: e.activation(out=zs[:, i, blk * 256:(blk + 1) * 256],
                                                                in_=self.pb(b)[:, 0:256], func=AF.Silu,
                                                                scale=rstd_col[:, i:i + 1]),
                               r=[("ps", b), "rstd_col"], w=["zs"])
                    self.add_job(self.wload(Wi, 0, KC, g * 512 + blk * 256, 256), comp)
                for blk in range(2):
                    def comp(wb, key, blk=blk, g=g):
                        wv = wb[:, 0:KC * 256].rearrange("p (kc e) -> p kc e", e=256)
                        for ec in range(2):
                            cc = blk * 2 + ec
                            q = g * 4 + cc
                            b = self.bank()
                            for kc in range(KC):
                                self.mm(self.pb(b)[:, :], wv[:, kc, ec * 128:(ec + 1) * 128], hnT[:, kc, :], kc == 0,
                                        kc == KC - 1, r=[key, "hnT"], w=[("ps", b)])
                            xo = xsTc[q % 3]
                            xk = ("xsTc", q % 3)
                            conv_chunk(q, b, xo, xk)

                            def trx(xo=xo, xk=xk, cc=cc):
                                b2 = self.bank()
                                pv = self.pbf(b2)[:, 0:512].rearrange("p (i c) -> p i c", c=128)
                                for i in range(4):
                                    self.tr(pv[:, i, :], xo[:, i * 128:(i + 1) * 128], self.ident_b[:, :], r=[xk],
                                            w=[("ps", b2)])
                                V(lambda e, pv=pv, cc=cc: e.tensor_copy(out=xs[:, :, cc * 128:(cc + 1) * 128], in_=pv),
                                  r=[("ps", b2)], w=["xs"])
                            defer(trx)
                    self.add_job(self.wload(Wi, 0, KC, 4096 + g * 512 + blk * 256, 256), comp)
                for which in range(2):
                    def comp(wb, key, which=which, g=g):
                        wv = wb[:, 0:KC * 128].rearrange("p (kc e) -> p kc e", e=128)
                        q = 32 + which * 8 + g
                        b = self.bank()
                        for kc in range(KC):
                            self.mm(self.pb(b)[:, :], wv[:, kc, :], hnT[:, kc, :], kc == 0, kc == KC - 1,
                                    r=[key, "hnT"], w=[("ps", b)])
                        if which == 0:
                            conv_chunk(q, b, BT, "BT")

                            def trb():
                                b2 = self.bank()
                                pv = self.pbf(b2)[:, 0:512].rearrange("p (i c) -> p i c", c=128)
                                for i in range(4):
                                    self.tr(pv[:, i, :], BT[:, i * 128:(i + 1) * 128], self.ident_b[:, :], r=["BT"],
                                            w=[("ps", b2)])
                                V(lambda e, pv=pv: e.tensor_copy(out=Btm, in_=pv), r=[("ps", b2)], w=["Btm"])
                            defer(trb)
                        else:
                            conv_chunk(q, b, CT, "CT")
                    self.add_job(self.wload(Wi, 0, KC, 8192 + which * 1024 + g * 128, 128), comp)

                def rec(wb, key, g=g, j=j):
                    gs = slice(g * 8, (g + 1) * 8)
                    bc8 = lambda ap: ap.unsqueeze(2).to_broadcast([128, 8, 64])
                    flush()
                    Sc(lambda e: e.activation(out=stb, in_=stf[:, g, :], func=AF.Copy), r=[("stf", g)], w=["stb"])
                    LA = 6
                    steps = [(c, hl) for c in range(4) for hl in range(8)]

                    def cbm_pre(c):
                        cs = slice(c * 128, (c + 1) * 128)
                        pi = c % 2
                        b1 = self.bank()
                        self.mm(self.pb(b1)[:, 0:128], BT[:, cs], CT[:, cs], True, True, r=["BT", "CT"], w=[("ps", b1)])
                        V(lambda e, b1=b1, pi=pi: e.tensor_tensor(out=cbmm[pi], in0=self.pb(b1)[:, 0:128],
                                                                  in1=self.tri_f[:, :], op=ALU.mult),
                          r=[("ps", b1)], w=[("cbmm", pi)])

                    def yoff(c):
                        cs = slice(c * 128, (c + 1) * 128)
                        ya = y1[c % 2]
                        yk = ("y1", c % 2)
                        b2 = self.bank()
                        self.mm(self.pb(b2)[:, :], CT[:, cs], stb, True, True, r=["CT", "stb"], w=[("ps", b2)])
                        V(lambda e, b2=b2, ya=ya, c=c: e.tensor_tensor(out=hv(ya), in0=hv(self.pb(b2)[:, :]),
                                                                       in1=bc8(Tm["ecum"][:, c, gs]), op=ALU.mult),
                          r=[("ps", b2), "Tecum"], w=[yk])

                    def stageA(s_):
                        c, hl = steps[s_]
                        cs = slice(c * 128, (c + 1) * 128)
                        h = g * 8 + hl
                        p2 = s_ % 8
                        pi = c % 2
                        bE = self.bank()
                        self.mm(self.pb(bE)[:, 0:128], self.ident_f[0:64, h:h + 1].to_broadcast([64, 128]),
                                cumT[0:64, cs], True, False, r=["cumT"], w=[("ps", bE)])
                        self.mm(self.pb(bE)[:, 0:128], self.ident_b[:, :], self.neg_b[:, :], False, True, r=[],
                                w=[("ps", bE)])
                        Sc(lambda e, bE=bE, p2=p2, c=c, h=h: e.activation(
                            out=dec[p2], in_=self.pb(bE)[:, 0:128], func=AF.Exp, bias=Tm["ncum"][:, c, h:h + 1]),
                           r=[("ps", bE), "Tncum"], w=[("dec", p2)])
                        V(lambda e, p2=p2, pi=pi, c=c, h=h: e.scalar_tensor_tensor(
                            out=WT[p2], in0=dec[p2], scalar=Tm["dt"][:, c, h:h + 1], in1=cbmm[pi], op0=ALU.mult,
                            op1=ALU.mult), r=[("dec", p2), ("cbmm", pi), "Tdt"], w=[("WT", p2)])

                    def stageD(s_):
                        c, hl = steps[s_]
                        p2 = s_ % 8
                        self.mm(self.pb(6 + c % 2)[:, hl * 64:(hl + 1) * 64], WT[p2], xs[:, c, hl * 64:(hl + 1) * 64],
                                True, True, r=[("WT", p2), "xs"], w=[("ps", 6 + c % 2)])

                    def st_pre(c):
                        pi = c % 2
                        V(lambda e, pi=pi, c=c: e.tensor_tensor(out=hv(xw[pi]), in0=hv(xs[:, c, :]),
                                                                in1=bc8(Tm["te"][:, c, gs]), op=ALU.mult),
                          r=["xs", "Tte"], w=[("xw", pi)])

                    def st_mm(c):
                        pi = c % 2
                        b4 = self.bank()
                        self.mm(self.pb(b4)[:, :], Btm[:, c, :], xw[pi], True, True, r=["Btm", ("xw", pi)],
                                w=[("ps", b4)])
                        V(lambda e, c=c: e.tensor_tensor(out=hv(stf[:, g, :]), in0=hv(stf[:, g, :]),
                                                         in1=bc8(Tm["ecl"][:, c, gs]), op=ALU.mult),
                          r=[("stf", g), "Tecl"], w=[("stf", g)])
                        V(lambda e, b4=b4: e.tensor_tensor(out=stf[:, g, :], in0=stf[:, g, :], in1=self.pb(b4)[:, :],
                                                           op=ALU.add), r=[("stf", g), ("ps", b4)], w=[("stf", g)])
                        if c < 3:
                            Sc(lambda e: e.activation(out=stb, in_=stf[:, g, :], func=AF.Copy), r=[("stf", g)],
                               w=["stb"])

                    def post_y(c):
                        pi = c % 2
                        ya = y1[pi]
                        yk = ("y1", pi)
                        bd = 6 + c % 2
                        V(lambda e, ya=ya: e.tensor_tensor(out=ya, in0=ya, in1=self.pb(bd)[:, :], op=ALU.add),
                          r=[("ps", bd), yk], w=[yk])
                        V(lambda e, c=c: e.tensor_tensor(out=hv(y2), in0=hv(xs[:, c, :]), in1=bc8(d_bc[:, gs]),
                                                         op=ALU.mult), r=["xs", "d_bc"], w=["y2"])
                        V(lambda e, ya=ya: e.tensor_tensor(out=ya, in0=ya, in1=y2, op=ALU.add), r=[yk, "y2"], w=[yk])
                        V(lambda e, ya=ya, c=c: e.tensor_tensor(out=ya, in0=ya, in1=zs[:, c, :], op=ALU.mult),
                          r=[yk, "zs"], w=[yk])
                        Sc(lambda e, ya=ya: e.activation(out=junk, in_=ya, func=AF.Square, accum_out=sm["ssq"]),
                           r=[yk], w=["junk", "ssq"])
                        self.rstd_from(sm["ssq"], "ssq", sm["rs"], "rs", sm["ssq"], "ssq", 1.0 / 512.0)
                        V(lambda e, ya=ya, c=c: e.tensor_scalar(out=zs[:, c, :], in0=ya, scalar1=sm["rs"][:, 0:1],
                                                                scalar2=None, op0=ALU.mult), r=[yk, "rs"], w=["zs"])

                    cbm_pre(0)
                    yoff(0)
                    for s_ in range(32 + LA):
                        if s_ < 32:
                            c, hl = steps[s_]
                            if hl == 0 and c + 1 < 4:
                                cbm_pre(c + 1)
                            stageA(s_)
                        sd = s_ - LA
                        if sd >= 0:
                            stageD(sd)
                            c, hl = steps[sd]
                            if hl == 7:
                                st_pre(c)
                                post_y(c)
                            if hl == 1 and c >= 1:
                                st_mm(c - 1)
                            if hl == 5 and c >= 1:
                                yoff(c)
                    st_mm(3)
                    for cc in range(4):
                        b = self.bank()
                        pv = self.pbf(b)[:, 0:512]
                        for i in range(4):
                            self.tr(pv[:, i * 128:(i + 1) * 128], zs[:, i, cc * 128:(cc + 1) * 128], self.ident_b[:, :],
                                    r=["zs"], w=[("ps", b)])
                        ec = g * 4 + cc
                        Sc(lambda e, pv=pv, ec=ec: e.activation(out=ynT[:, ec, :], in_=pv, func=AF.Copy,
                                                                scale=nwcol[:, ec:ec + 1]),
                           r=[("ps", b), "nwcol"], w=[("ynT", ec)])
                self.add_job(None, rec)

            if self.dbg and j == 0 and self.stop == "A1only_t1":
                def dump(wb, key):
                    for nm, t, shp, dt_ in [("dtT", dtT, [64, TT], F32), ("cumT", cumT, [64, TT], F32),
                                            ("Tdt", Tm["dt"], [128, 4, 64], F32), ("Tcum", Tm["cum"], [128, 4, 64], F32),
                                            ("Tcl", Tm["cl"], [128, 4, 64], F32), ("Tte", Tm["te"], [128, 4, 64], F32),
                                            ("xs", xs, [128, 4, 512], BF16), ("BT", BT, [128, TT], BF16),
                                            ("CT", CT, [128, TT], BF16), ("Btm", Btm, [128, 4, 128], BF16),
                                            ("zs", zs, [128, 4, 512], BF16), ("ynT", ynT, [128, 32, TT], BF16),
                                            ("stf", stf, [128, 8, 512], F32), ("hnT", hnT, [128, KC, TT], BF16),
                                            ("rstd_bc", rstd_bc, [128, TT], F32), ("dec0", dec[0], [128, 128], F32),
                                            ("dec1", dec[1], [128, 128], F32), ("cbmm1", cbmm[1], [128, 128], F32),
                                            ("WT1", WT[1], [128, 128], BF16), ("y11", y1[1], [128, 512], F32),
                                            ("y2", y2, [128, 512], F32), ("Tncum", Tm["ncum"], [128, 4, 64], F32)]:
                        o = self.dbg_tensor("dbg_" + nm, shp, dt_)
                        self.dma(o, t, r=["cumT", "dtT", "Tdt", "Tcum", "Tcl", "Tte", "xs", "BT", "CT", "Btm", "zs",
                                          "ynT", "stf", "hnT", "rstd_bc", "dec", "cbmm", "WT", "y1", "y2", "Tncum"], w=[("dbgw", nm)])
                self.add_job(None, dump)
            for dc in range(KC):
                def comp(wb, key, dc=dc, j=j):
                    wv = wb[:, 0:32 * 128].rearrange("p (kc e) -> p kc e", e=128)
                    b = self.bank()
                    for ec in range(32):
                        self.mm(self.pb(b)[:, :], wv[:, ec, :], ynT[:, ec, :], ec == 0, ec == 31, r=[key, "ynT"],
                                w=[("ps", b)])
                    self.outproj_chunk(1, j, dc, self.hcur, b)
                    if dc == KC - 1:
                        self.moe_route(1, j)
                self.add_job(self.wload(d["ssd_w_out"], 0, 32, dc * 128, 128), comp)
        self.run_jobs()

    def phase_B(self, l):
        d = self.din
        A = self.A
        V, Sc = self.V, self.Sc
        xr = [A.alloc([128, 3, D], BF16) for _ in range(2)]
        xbT = A.alloc([128, KC, CAP], BF16)
        sg = A.alloc([128, 4, CAP], F32)
        hidT = A.alloc([128, 4, CAP], BF16)
        yrow = [A.alloc([128, D], F32) for _ in range(3)]
        sc_keys = [("xs_sc", i) for i in range(64)]
        extra = [A.alloc([128, 4096], BF16) for _ in range(10)]
        ne = NE if self.stop not in ("B0e2",) else 2
        for e_ in range(ne):
            xi = e_ % 2

            def load_x(en):
                for (r0, rn) in RB:
                    rb = r0 // 128
                    self.dma(xr[en % 2][0:rn, rb, :], self.xslots[en * CAP + r0:en * CAP + r0 + rn, :],
                             r=sc_keys, w=[("xr", en % 2)])

            def prep(wb, key, e_=e_, xi=xi):
                if e_ == 0:
                    load_x(0)
                if e_ + 1 < ne:
                    load_x(e_ + 1)
                for q4 in range(4):
                    for (r0, rn) in RB:
                        rb = r0 // 128
                        b = self.bank()
                        pv = self.pbf(b)[:, 0:512].rearrange("p (a r) -> p a r", r=128)
                        for a in range(4):
                            dc = q4 * 4 + a
                            self.tr(pv[:, a, 0:rn], xr[xi][0:rn, rb, dc * 128:(dc + 1) * 128], self.ident_b[0:rn, 0:rn],
                                    r=[("xr", xi)], w=[("ps", b)])
                        eng = Sc if (q4 + rb) % 2 == 0 else V
                        if eng is Sc:
                            Sc(lambda e, pv=pv, q4=q4, r0=r0, rn=rn: e.activation(
                                out=xbT[:, q4 * 4:(q4 + 1) * 4, r0:r0 + rn], in_=pv[:, :, 0:rn], func=AF.Copy),
                               r=[("ps", b)], w=["xbT"])
                        else:
                            V(lambda e, pv=pv, q4=q4, r0=r0, rn=rn: e.tensor_copy(
                                out=xbT[:, q4 * 4:(q4 + 1) * 4, r0:r0 + rn], in_=pv[:, :, 0:rn]),
                              r=[("ps", b)], w=["xbT"])
            self.add_job(None, prep)
            for which in range(2):
                W = d["moe_wg"] if which == 0 else d["moe_wu"]
                for blk in range(2):
                    def comp(wb, key, blk=blk, which=which):
                        wv = wb[:, 0:KC * 256].rearrange("p (kc e) -> p kc e", e=256)
                        for fc in range(2):
                            m = blk * 2 + fc
                            b = self.bank()
                            for kc in range(KC):
                                self.mm(self.pb(b)[:, 0:CAP], wv[:, kc, fc * 128:(fc + 1) * 128], xbT[:, kc, :],
                                        kc == 0, kc == KC - 1, r=[key, "xbT"], w=[("ps", b)])
                            if which == 0:
                                Sc(lambda e, b=b, m=m: e.activation(out=sg[:, m, :], in_=self.pb(b)[:, 0:CAP],
                                                                    func=AF.Silu), r=[("ps", b)], w=[("sg", m)])
                            else:
                                V(lambda e, b=b, m=m: e.tensor_tensor(out=hidT[:, m, :], in0=self.pb(b)[:, 0:CAP],
                                                                      in1=sg[:, m, :], op=ALU.mult),
                                  r=[("ps", b), ("sg", m)], w=[("hidT", m)])
                    self.add_job(self.wload(W[l, e_], 0, KC, blk * 256, 256), comp)
            for blk in range(2):
                def comp(wb, key, blk=blk, e_=e_):
                    wv = wb[:, 0:4 * 1024].rearrange("p (kc e) -> p kc e", e=1024)
                    for (r0, rn) in RB:
                        rb = r0 // 128
                        for nb_ in range(2):
                            b = self.bank()
                            for m in range(4):
                                self.mm(self.pb(b)[0:rn, :], hidT[:, m, r0:r0 + rn], wv[:, m, nb_ * 512:(nb_ + 1) * 512],
                                        m == 0, m == 3, r=[key, "hidT"], w=[("ps", b)])
                            c0 = blk * 1024 + nb_ * 512
                            if nb_ == 0:
                                Sc(lambda e, b=b, rb=rb, rn=rn, c0=c0: e.activation(
                                    out=yrow[rb][0:rn, c0:c0 + 512], in_=self.pb(b)[0:rn, :], func=AF.Copy),
                                   r=[("ps", b)], w=[("yrow", rb)])
                            else:
                                V(lambda e, b=b, rb=rb, rn=rn, c0=c0: e.tensor_copy(
                                    out=yrow[rb][0:rn, c0:c0 + 512], in_=self.pb(b)[0:rn, :]),
                                  r=[("ps", b)], w=[("yrow", rb)])
                        if blk == 1:
                            self.dma(self.yslots[e_ * CAP + r0:e_ * CAP + r0 + rn, :], yrow[rb][0:rn, :],
                                     r=[("yrow", rb)], w=[("ys", e_)])
                self.add_job(self.wload(d["moe_wd"][l, e_], 0, 4, blk * 1024, 1024), comp)
        self.run_jobs(extra=extra)

    def phase_C(self, l):
        A = self.A
        V, Sc = self.V, self.Sc
        y = [[A.alloc([128, D], F32) for _ in range(2)] for _ in range(2)]
        moe = [A.alloc([128, D], F32) for _ in range(2)]
        hmb = [A.alloc([128, KC, 128], F32) for _ in range(2)]
        hn = [A.alloc([128, KC, 128], F32) for _ in range(2)]
        sq = [A.alloc([128, KC, 128], BF16) for _ in range(2)]
        rs = A.alloc([128, 128], F32)
        ys_keys = [("ys", e) for e in range(NE)]
        for p_ in range(2):
            for k in range(2):
                V(lambda e, p_=p_, k=k: e.memset(y[p_][k], 0.0), r=[], w=[("y", p_, k)])
        last = (l == 1)
        dst = self.outT if last else self.hcur
        nsub = 32 if self.stop not in ("C0s2",) else 2
        for ti in range(nsub):
            p_ = ti % 2
            ts = slice(ti * 128, (ti + 1) * 128)
            for k in range(2):
                self.P.op("gpsimd", lambda e, p_=p_, k=k, ti=ti: e.indirect_dma_start(
                    out=y[p_][k][:, :], out_offset=None, in_=self.yslots[:, :],
                    in_offset=bass.IndirectOffsetOnAxis(ap=self.slots_i[:, ti, k:k + 1], axis=0),
                    bounds_check=self.bcreg(e), oob_is_err=False), r=ys_keys, w=[("y", p_, k)], dma=True)
            self.dma(hmb[p_], self.hmixT[:, ts].rearrange("(kc p) t -> p kc t", p=128), r=[("hmixT", ti // 4)],
                     w=[("hmb", p_)])
            V(lambda e, p_=p_, ti=ti: e.tensor_scalar(out=moe[p_], in0=y[p_][0], scalar1=self.gates[:, ti, 0:1],
                                                     scalar2=None, op0=ALU.mult), r=[("y", p_, 0)], w=[("moe", p_)])
            V(lambda e, p_=p_, ti=ti: e.scalar_tensor_tensor(out=moe[p_], in0=y[p_][1], scalar=self.gates[:, ti, 1:2],
                                                            in1=moe[p_], op0=ALU.mult, op1=ALU.add),
              r=[("y", p_, 1), ("moe", p_)], w=[("moe", p_)])
            for q4 in range(4):
                b = self.bank()
                pv = self.pb(b)[:, :].rearrange("p (a t) -> p a t", t=128)
                for a in range(4):
                    dc = q4 * 4 + a
                    self.tr(pv[:, a, :], moe[p_][:, dc * 128:(dc + 1) * 128], self.ident_f[:, :],
                            r=[("moe", p_)], w=[("ps", b)])
                V(lambda e, pv=pv, q4=q4, p_=p_: e.tensor_tensor(out=hn[p_][:, q4 * 4:(q4 + 1) * 4, :], in0=pv,
                                                                in1=hmb[p_][:, q4 * 4:(q4 + 1) * 4, :], op=ALU.add),
                  r=[("ps", b), ("hmb", p_)], w=[("hn", p_)])
            if last:
                Sc(lambda e, p_=p_: e.activation(out=sq[p_], in_=hn[p_], func=AF.Square), r=[("hn", p_)],
                   w=[("sqc", p_)])
                b = self.bank()
                for kc in range(KC):
                    self.mm(self.pb(b)[:, 0:128], self.ones_b[:, :], sq[p_][:, kc, :], kc == 0, kc == KC - 1,
                            r=[("sqc", p_)], w=[("ps", b)])
                self.rstd_from(self.pb(b)[:, 0:128], ("ps", b), rs, "rsC", rs, "rsC", 1.0 / D)
                for kc in range(KC):
                    V(lambda e, kc=kc, p_=p_: e.scalar_tensor_tensor(
                        out=hn[p_][:, kc, :], in0=hn[p_][:, kc, :], scalar=self.fnw[:, kc:kc + 1], in1=rs,
                        op0=ALU.mult, op1=ALU.mult), r=[("hn", p_), "rsC"], w=[("hn", p_)])
            self.dma(dst[:, ts].rearrange("(kc p) t -> p kc t", p=128), hn[p_], r=[("hn", p_)], w=[("dst", ti // 4)])

    def build(self):
        stop = self.stop
        self.phase_barrier()
        if stop.startswith("A1only"):
            self.phase_A1()
            return self.finish()
        self.phase_A0()
        if stop.startswith("A0"):
            return self.finish()
        self.phase_barrier()
        self.phase_B(0)
        if stop.startswith("B0"):
            return self.finish()
        self.phase_barrier()
        self.phase_C(0)
        if stop.startswith("C0"):
            return self.finish()
        self.phase_barrier()
        self.phase_A1()
        if stop.startswith("A1"):
            return self.finish()
        self.phase_barrier()
        self.phase_B(1)
        self.phase_barrier()
        self.phase_C(1)
        return self.finish()

    def finish(self):
        if self.dbg:
            self.dma(self.dbg_slots, self.slots_i[:].rearrange("p a b -> p (a b)"), r=["slots"], w=["dbg1"])
            self.dma(self.dbg_gates, self.gates[:].rearrange("p a b -> p (a b)"), r=["gates"], w=["dbg2"])
        self.P.emit()
        return self.nc


def _col(v, n):
    return np.ascontiguousarray(np.asarray(v, np.float32).reshape(n, 128).T)


def _shared_inputs(inp):
    f = lambda k: np.asarray(inp[k], np.float32)
    sh = {}
    sh["nmw"] = np.ascontiguousarray(np.concatenate([_col(f("norm_mix_w")[0], 16), _col(f("norm_mix_w")[1], 16)], axis=1))
    sh["nfw"] = np.ascontiguousarray(np.concatenate([_col(f("norm_ffn_w")[0], 16), _col(f("norm_ffn_w")[1], 16)], axis=1))
    sh["fnw"] = _col(f("final_norm_w"), 16)
    sh["ml_w_in"] = np.ascontiguousarray(f("ml_w_in")[0])
    sh["ml_bi"] = np.ascontiguousarray(f("ml_b_i")[0].reshape(4, 1))
    sh["ml_bf"] = np.ascontiguousarray(f("ml_b_f")[0].reshape(4, 1))
    sh["ml_nw_col"] = _col(f("ml_norm_w")[0], 16)
    sh["ml_w_out"] = np.ascontiguousarray(f("ml_w_out")[0])
    sh["ssd_w_in"] = np.ascontiguousarray(f("ssd_w_in")[0])
    sh["conv_w"] = np.ascontiguousarray(f("ssd_conv_w")[0].T.reshape(48, 128, 4).transpose(1, 0, 2))
    sh["conv_b"] = _col(f("ssd_conv_b")[0], 48)
    sh["dtb_col"] = np.ascontiguousarray(f("ssd_dt_bias")[0].reshape(64, 1))
    sh["dtb_bc"] = np.ascontiguousarray(np.broadcast_to(f("ssd_dt_bias")[0][None, :], (128, 64)))
    sh["alog_col"] = np.ascontiguousarray(f("ssd_a_log")[0].reshape(64, 1))
    sh["d_bc"] = np.ascontiguousarray(np.broadcast_to(f("ssd_d")[0][None, :], (128, 64)))
    sh["ssd_nw_col"] = _col(f("ssd_norm_w")[0], 32)
    sh["ssd_w_out"] = np.ascontiguousarray(f("ssd_w_out")[0])
    sh["moe_wr"] = np.ascontiguousarray(np.concatenate([f("moe_w_group"), f("moe_w_expert")], axis=-1))
    br = np.concatenate([f("moe_b_group"), f("moe_b_expert")], axis=-1)
    sh["moe_br_bc"] = np.ascontiguousarray(np.broadcast_to(br[:, None, :], (2, 128, 72)))
    sh["moe_wg"] = f("moe_w_gate")
    sh["moe_wu"] = f("moe_w_up")
    sh["moe_wd"] = f("moe_w_down")
    s_ = np.arange(128)[:, None]
    t_ = np.arange(128)[None, :]
    sh["c_ident"] = np.eye(128, dtype=np.float32)
    sh["c_tri"] = (s_ <= t_).astype(np.float32)
    sh["c_tris"] = (s_ < t_).astype(np.float32)
    sh["c_neg"] = np.where(s_ <= t_, 0.0, -30000.0).astype(np.float32)
    sh["c_ebase"] = np.ascontiguousarray(np.broadcast_to((np.arange(64) * CAP).astype(np.float32)[None, :], (128, 64)))
    rm = np.ones((64, 512), np.float32)
    rm[:, ::128] = 0.0
    sh["c_reset"] = rm
    return sh


def core_inputs(inp, b, sh=None):
    sh = sh if sh is not None else _shared_inputs(inp)
    m = dict(sh)
    m["xT"] = np.ascontiguousarray(np.asarray(inp["x"], np.float32)[b].T)
    return m


_NC_CACHE = {}


def kernel(**inputs):
    if "nc" not in _NC_CACHE:
        _NC_CACHE["nc"] = Builder(stop="all", dbg=False).build()
    nc = _NC_CACHE["nc"]
    sh = _shared_inputs(inputs)
    in_maps = [core_inputs(inputs, b, sh) for b in range(8)]
    res = run_bass_kernel_spmd(nc, in_maps, core_ids=list(range(8)))
    out = np.stack([np.ascontiguousarray(r["outT"].T) for r in res.results], axis=0)
    return out.astype(np.float32)
```

```python
import numpy as np
from contextlib import ExitStack
import concourse.bass as bass
import concourse.mybir as mybir
from concourse.bass_utils import run_bass_kernel_spmd

F32 = mybir.dt.float32
BF16 = mybir.dt.bfloat16
I32 = mybir.dt.int32
AF = mybir.ActivationFunctionType
ALU = mybir.AluOpType
AX = mybir.AxisListType

ROT = 30000
NDMASEM = 8

D = 2048
S = 4096
KC = 16
TT = 512
NT = S // TT
NE = 64
CAP = 320
RB = [(0, 128), (128, 128), (256, 64)]
NSLOT = NE * CAP
EPS = 1e-6
ML_IN = 6152
SSD_IN = 10304


class Prog:
    def __init__(self, nc):
        self.nc = nc
        self.ops = []
        self.stack = ExitStack()
        self.st = {}
        self.names = {}

    def sb(self, name, shape, dt):
        return self.stack.enter_context(self.nc.sbuf_tensor("s_" + name, list(shape), dt))

    def ps(self, name, shape, dt):
        return self.stack.enter_context(self.nc.psum_tensor(name, list(shape), dt))

    @staticmethod
    def _norm(k):
        return k if isinstance(k, tuple) else (k,)

    def _conf(self, k):
        name = k[0]
        ks = self.names.get(name, ())
        if len(k) == 1:
            return list(ks)
        out = []
        if k in ks:
            out.append(k)
        if (name,) in ks:
            out.append((name,))
        return out

    def op(self, eng, fn, r=(), w=(), dma=False):
        oid = len(self.ops)
        deps = {}
        r = [self._norm(k) for k in r]
        w = [self._norm(k) for k in w]
        for k in r:
            for c in self._conf(k):
                lw = self.st[c][0]
                if lw is not None:
                    deps[lw] = True
        for k in w:
            for c in self._conf(k):
                s = self.st[c]
                if s[0] is not None:
                    deps.setdefault(s[0], False)
                for rd in s[1]:
                    deps.setdefault(rd, False)
        for k in r:
            if k not in self.st:
                self.st[k] = [None, []]
                self.names.setdefault(k[0], set()).add(k)
            self.st[k][1].append(oid)
        for k in w:
            if k not in self.st:
                self.st[k] = [None, []]
                self.names.setdefault(k[0], set()).add(k)
            if len(k) == 1:
                for c in self._conf(k):
                    self.st[c] = [oid, []]
            self.st[k] = [oid, []]
        self.ops.append(dict(eng=eng, fn=fn, deps=deps, dma=dma, bar=False))
        return oid

    def barrier(self):
        self.ops.append(dict(eng=None, fn=None, deps={}, dma=False, bar=True))
        self.st = {}
        self.names = {}

    def emit(self):
        nc = self.nc
        ops = self.ops
        engs = ["tensor", "vector", "scalar", "gpsimd", "sync"]
        cnt_c = {e: 0 for e in engs}
        cnt_d = {e: 0 for e in engs}
        for o in ops:
            if o["bar"]:
                continue
            e = o["eng"]
            if o["dma"]:
                o["n"] = cnt_d[e]
                cnt_d[e] += 1
            else:
                o["n"] = cnt_c[e]
                cnt_c[e] += 1
        sems_c = {}
        for e in engs:
            nrot = (cnt_c[e] + ROT - 1) // ROT
            sems_c[e] = [self.stack.enter_context(nc.semaphore(f"c_{e}_{i}")) for i in range(nrot)]
        sems_d = {}
        for e in engs:
            ns = min(NDMASEM, cnt_d[e])
            sems_d[e] = [self.stack.enter_context(nc.semaphore(f"d_{e}_{i}")) for i in range(ns)]

        def signal(o):
            if o["dma"]:
                return sems_d[o["eng"]][o["n"] % NDMASEM], 16 * (o["n"] // NDMASEM + 1)
            return sems_c[o["eng"]][o["n"] // ROT], o["n"] % ROT + 1

        per_eng = {e: [] for e in engs}
        last_dma = {e: {} for e in engs}
        last_c = {e: None for e in engs}
        pend_bar = {e: [] for e in engs}
        for oid, o in enumerate(ops):
            if o["bar"]:
                ws = []
                for e2 in engs:
                    if last_c[e2] is not None:
                        ws.append(signal(ops[last_c[e2]]))
                    for slot, d in last_dma[e2].items():
                        ws.append(signal(ops[d]))
                for e2 in engs:
                    pend_bar[e2] = list(ws)
                continue
            e = o["eng"]
            waits = list(pend_bar[e])
            pend_bar[e] = []
            for d, raw in o["deps"].items():
                p = ops[d]
                if (not p["dma"]) and (not o["dma"]) and p["eng"] == e:
                    if e == "tensor" or not raw:
                        continue
                waits.append(signal(p))
            if o["dma"]:
                slot = o["n"] % NDMASEM
                if slot in last_dma[e]:
                    waits.append(signal(ops[last_dma[e][slot]]))
                last_dma[e][slot] = oid
            else:
                last_c[e] = oid
            o["waits"] = waits
            per_eng[e].append(o)

        with nc.Block() as block:
            def run(e, eng):
                known = {}
                for o in per_eng[e]:
                    for sem, val in o["waits"]:
                        key = id(sem)
                        if known.get(key, 0) >= val:
                            continue
                        known[key] = val
                        eng.wait_ge(sem, val)
                    ins = o["fn"](eng)
                    sem, val = signal(o)
                    ins.then_inc(sem, 16 if o["dma"] else 1)
                for i, sem in enumerate(sems_d[e]):
                    n = cnt_d[e]
                    k = (n - i + NDMASEM - 1) // NDMASEM
                    if k > 0:
                        eng.wait_ge(sem, 16 * k)

            if per_eng["sync"]:
                @block.sync
                def _(eng):
                    run("sync", eng)
            if per_eng["gpsimd"]:
                @block.gpsimd
                def _(eng):
                    run("gpsimd", eng)
            if per_eng["scalar"]:
                @block.scalar
                def _(eng):
                    run("scalar", eng)
            if per_eng["vector"]:
                @block.vector
                def _(eng):
                    run("vector", eng)
            if per_eng["tensor"]:
                @block.tensor
                def _(eng):
                    run("tensor", eng)
        self.stack.close()


class Arena:
    def __init__(self, tile, nwords):
        self.t = tile
        self.n = nwords
        self.off = 0

    def reset(self):
        self.off = 0

    def alloc(self, shape, dt, parts=None):
        parts = shape[0]
        free = int(np.prod(shape[1:]))
        words = free if dt in (F32, I32) else (free + 1) // 2
        words = (words + 7) // 8 * 8
        assert self.off + words <= self.n, ("arena overflow", self.off, words, self.n)
        ap = self.t[0:parts, self.off:self.off + words]
        self.off += words
        if dt != F32:
            ap = ap.bitcast(dt)
        ap = ap[:, 0:free]
        if len(shape) == 3:
            ap = ap.rearrange("p (a b) -> p a b", b=shape[2])
        elif len(shape) == 4:
            ap = ap.rearrange("p (a b c) -> p a b c", b=shape[2], c=shape[3])
        return ap


class Builder:
    def __init__(self, stop="all", dbg=False):
        self.stop = stop
        self.dbg = dbg
        nc = bass.Bass("TRN2", target_bir_lowering=False)
        self.nc = nc
        self.P = Prog(nc)
        self.din = {}
        self.jobs = []
        self._decl_io()
        self._alloc_static()

    def _in(self, name, shape, dt=F32):
        self.din[name] = self.nc.dram_tensor(name, list(shape), dt, kind="ExternalInput").ap()
        return self.din[name]

    def _decl_io(self):
        nc = self.nc
        self._in("xT", [D, S])
        self._in("nmw", [128, 32])
        self._in("nfw", [128, 32])
        self._in("fnw", [128, 16])
        self._in("ml_w_in", [D, ML_IN])
        self._in("ml_bi", [4, 1])
        self._in("ml_bf", [4, 1])
        self._in("ml_nw_col", [128, 16])
        self._in("ml_w_out", [D, D])
        self._in("ssd_w_in", [D, SSD_IN])
        self._in("conv_w", [128, 48, 4])
        self._in("conv_b", [128, 48])
        self._in("dtb_col", [64, 1])
        self._in("dtb_bc", [128, 64])
        self._in("alog_col", [64, 1])
        self._in("d_bc", [128, 64])
        self._in("ssd_nw_col", [128, 32])
        self._in("ssd_w_out", [4096, D])
        self._in("moe_wr", [2, D, 72])
        self._in("moe_br_bc", [2, 128, 72])
        self._in("moe_wg", [2, NE, D, 512])
        self._in("moe_wu", [2, NE, D, 512])
        self._in("moe_wd", [2, NE, 512, D])
        self._in("c_ident", [128, 128])
        self._in("c_tri", [128, 128])
        self._in("c_tris", [128, 128])
        self._in("c_neg", [128, 128])
        self._in("c_ebase", [128, 64])
        self._in("c_reset", [64, 512])
        self.outT = nc.dram_tensor("outT", [D, S], F32, kind="ExternalOutput").ap()
        kw = dict(kind="ExternalOutput") if self.dbg else {}
        self.hmixT = nc.dram_tensor("hmixT", [D, S], F32, **kw).ap()
        if self.stop.startswith("A1only"):
            self.hcur = nc.dram_tensor("hcur", [D, S], F32, kind="ExternalInput").ap()
        else:
            self.hcur = nc.dram_tensor("hcur", [D, S], F32, **kw).ap()
        self.xslots = nc.dram_tensor("xslots", [NSLOT, D], BF16, **kw).ap()
        self.yslots = nc.dram_tensor("yslots", [NSLOT, D], F32, **kw).ap()
        if self.dbg:
            self.dbg_slots = nc.dram_tensor("dbg_slots", [128, 64], I32, kind="ExternalOutput").ap()
            self.dbg_gates = nc.dram_tensor("dbg_gates", [128, 64], F32, kind="ExternalOutput").ap()
        self.dbg_out = {}

    def dbg_tensor(self, name, shape, dt=F32):
        t = self.nc.dram_tensor(name, list(shape), dt, kind="ExternalOutput").ap()
        self.dbg_out[name] = t
        return t

    def _alloc_static(self):
        P = self.P
        self.psum = [P.ps(f"psb{i}", [128, 512], F32) for i in range(8)]
        self.bank_i = 0
        self.ident_f = P.sb("ident_f", [128, 128], F32)
        self.ident_b = P.sb("ident_b", [128, 128], BF16)
        self.ones_b = P.sb("ones_b", [128, 128], BF16)
        self.ones_f = P.sb("ones_f", [128, 128], F32)
        self.tri_f = P.sb("tri_f", [128, 128], F32)
        self.tris_b = P.sb("tris_b", [128, 128], BF16)
        self.neg_b = P.sb("neg_b", [128, 128], BF16)
        self.ebase = P.sb("ebase", [128, 64], F32)
        self.nmw = P.sb("nmw", [128, 32], F32)
        self.nfw = P.sb("nfw", [128, 32], F32)
        self.fnw = P.sb("fnw", [128, 16], F32)
        self.NWB = 4
        self.wbuf = [P.sb(f"wbuf{i}", [128, 4096], BF16) for i in range(self.NWB)]
        self.wb_i = 0
        self.slots_i = P.sb("slots_i", [128, 32, 2], I32)
        self.gates = P.sb("gates", [128, 32, 2], F32)
        self.carry_bc = P.sb("carry_bc", [128, 64], F32)
        self.wr_f = P.sb("wr_f", [128, 16, 72], F32)
        self.br_bc = P.sb("br_bc", [128, 72], F32)
        AW = 41984
        self.arena_t = P.sb("arena", [128, AW], F32)
        self.A = Arena(self.arena_t, AW)
        d = self.din
        o = P.op
        o("sync", lambda e: e.dma_start(out=self.ident_f[:], in_=d["c_ident"]), w=["ident_f"], dma=True)
        o("gpsimd", lambda e: e.dma_start(out=self.ident_b[:], in_=d["c_ident"]), w=["ident_b"], dma=True)
        o("sync", lambda e: e.dma_start(out=self.tri_f[:], in_=d["c_tri"]), w=["tri_f"], dma=True)
        o("gpsimd", lambda e: e.dma_start(out=self.tris_b[:], in_=d["c_tris"]), w=["tris_b"], dma=True)
        o("gpsimd", lambda e: e.dma_start(out=self.neg_b[:], in_=d["c_neg"]), w=["neg_b"], dma=True)
        o("sync", lambda e: e.dma_start(out=self.ebase[:], in_=d["c_ebase"]), w=["ebase"], dma=True)
        o("sync", lambda e: e.dma_start(out=self.nmw[:], in_=d["nmw"]), w=["nmw"], dma=True)
        o("sync", lambda e: e.dma_start(out=self.nfw[:], in_=d["nfw"]), w=["nfw"], dma=True)
        o("sync", lambda e: e.dma_start(out=self.fnw[:], in_=d["fnw"]), w=["fnw"], dma=True)
        o("vector", lambda e: e.memset(self.ones_b[:], 1.0), w=["ones_b"])
        o("vector", lambda e: e.memset(self.ones_f[:], 1.0), w=["ones_f"])
        self.eps_col = P.sb("eps_col", [128, 1], F32)
        o("vector", lambda e: e.memset(self.eps_col[:], EPS), w=["eps_col"])
        self.static_keys = ["ident_f", "ident_b", "tri_f", "tris_b", "neg_b", "ebase", "nmw", "nfw", "fnw",
                            "ones_b", "ones_f"]

    def bank(self):
        i = self.bank_i
        self.bank_i = (self.bank_i + 1) % 6
        return i

    def pb(self, i):
        return self.psum[i]

    def pbf(self, i):
        return self.psum[i][:].bitcast(BF16)

    def mm(self, out, lhsT, rhs, start, stop, r, w):
        self.P.op("tensor", lambda e: e.matmul(out, lhsT=lhsT, rhs=rhs, start=start, stop=stop), r=r, w=w)

    def tr(self, out, in_, ident, r, w):
        self.P.op("tensor", lambda e: e.transpose(out, in_, ident), r=r, w=w)

    def V(self, fn, r, w):
        self.P.op("vector", fn, r=r, w=w)

    def Sc(self, fn, r, w):
        self.P.op("scalar", fn, r=r, w=w)

    def dma(self, out, in_, r, w, q="sync"):
        self.P.op(q, lambda e: e.dma_start(out=out, in_=in_), r=r, w=w, dma=True)

    def bcreg(self, e):
        if getattr(self, "_bcreg", None) is None:
            self._bcreg = e.alloc_register("bcreg")
            e.reg_mov(self._bcreg, NSLOT - 1)
        return self._bcreg

    def phase_barrier(self):
        self.P.barrier()
        self.A.reset()

    def add_job(self, load, compute):
        self.jobs.append((load, compute))

    def run_jobs(self, extra=()):
        jobs = self.jobs
        self.jobs = []
        lj = [i for i, (l, c) in enumerate(jobs) if l is not None]
        wl = list(self.wbuf) + list(extra)
        depth = len(wl) - 1
        bufs = {}
        nxt = 0
        self.wb_i = 0

        def issue(k):
            i = lj[k]
            b = self.wb_i
            self.wb_i = (self.wb_i + 1) % len(wl)
            bufs[i] = b
            jobs[i][0](wl[b], ("wbuf", b))

        for k in range(min(depth, len(lj))):
            issue(k)
        nxt = min(depth, len(lj))
        for i, (l, c) in enumerate(jobs):
            if l is not None:
                if nxt < len(lj):
                    issue(nxt)
                    nxt += 1
                c(wl[bufs[i]], ("wbuf", bufs[i]))
            else:
                c(None, None)

    def wload(self, W, r0, kc_n, c0, ncols):
        def load(wb, key):
            src = W[r0:r0 + kc_n * 128, c0:c0 + ncols].rearrange("(kc p) e -> p kc e", p=128)
            dst = wb[:, 0:kc_n * ncols].rearrange("p (kc e) -> p kc e", e=ncols)
            self.P.op("gpsimd", lambda e: e.dma_start(out=dst, in_=src), w=[key], dma=True)
        return load

    def norm_in(self, src, j, nw, hnT, rstd_bc, rstd_col, tmp_bc):
        A = self
        bk = 6
        hch = [self.t_hch[i] for i in range(3)]
        for kc in range(KC):
            hb = hch[kc % 3]
            hk = ("hch", kc % 3)
            self.dma(hb, src[kc * 128:(kc + 1) * 128, j * TT:(j + 1) * TT], r=[], w=[hk])
            sq = self.t_sq[kc % 2]
            sk = ("sq", kc % 2)
            self.Sc(lambda e, hb=hb, sq=sq: e.activation(out=sq, in_=hb, func=AF.Square), r=[hk], w=[sk])
            self.mm(self.pb(bk)[:, :], self.ones_b[:, :], sq, kc == 0, kc == KC - 1, r=[sk], w=[("ps", bk)])
            self.V(lambda e, hb=hb, kc=kc: e.tensor_scalar(out=hnT[:, kc, :], in0=hb, scalar1=nw[:, kc:kc + 1],
                                                         scalar2=None, op0=ALU.mult), r=[hk], w=[("hnT", kc)])
        self.rstd_from(self.pb(bk)[:, :], ("ps", bk), rstd_bc, "rstd_bc", tmp_bc, "tmp_bc", 1.0 / D)
        b2 = self.bank()
        for i in range(4):
            self.mm(self.pb(b2)[:, i:i + 1], rstd_bc[0:1, i * 128:(i + 1) * 128], self.ones_f[0:1, 0:1], True, True,
                    r=["rstd_bc"], w=[("ps", b2)])
        self.V(lambda e: e.tensor_copy(out=rstd_col, in_=self.pb(b2)[:, 0:4]), r=[("ps", b2)], w=["rstd_col"])

    def rstd_from(self, ss, ss_key, out, out_key, tmp, tmp_key, inv_n):
        self.Sc(lambda e: e.activation(out=tmp, in_=ss, func=AF.Ln, scale=inv_n, bias=self.eps_col[0:ss.shape[0], 0:1]),
                r=[ss_key], w=[tmp_key])
        self.Sc(lambda e: e.activation(out=out, in_=tmp, func=AF.Exp, scale=-0.5), r=[tmp_key], w=[out_key])

    def alloc_common(self):
        A = self.A
        self.t_hch = [A.alloc([128, TT], F32) for _ in range(3)]
        self.t_sq = [A.alloc([128, TT], BF16) for _ in range(2)]
        self.t_hnT = A.alloc([128, KC, TT], BF16)
        self.t_rstd_bc = A.alloc([128, TT], F32)
        self.t_tmp_bc = A.alloc([128, TT], F32)
        self.t_rstd_col = A.alloc([128, 4], F32)
        self.t_rows = A.alloc([128, 4, D], BF16)
        self.t_hm = [A.alloc([128, TT], F32) for _ in range(2)]
        self.t_xnb = [A.alloc([128, TT], BF16) for _ in range(2)]
        self.t_r = {n: A.alloc([128, w], F32) for n, w in
                    [("ss2", 4), ("rs2", 4), ("lg", 72), ("gmax", 1), ("ngmax", 1), ("gex", 8), ("gsum", 1),
                     ("gw", 1), ("goh", 8), ("t88", 64), ("esel", 8), ("top8", 8), ("oh1", 8), ("oh2", 8),
                     ("dd", 1), ("ex", 1), ("ex1", 1), ("ew1", 1), ("ew2", 1), ("o641", 64), ("o642", 64),
                     ("cnt", 64), ("tt", 64), ("j64", 64), ("pos", 1), ("base", 1), ("ovf", 1), ("nov", 1),
                     ("slf", 1)]}
        self.t_Ab = A.alloc([128, 64], BF16)

    def moe_layer_setup(self, l):
        d = self.din
        self.dma(self.wr_f[:], d["moe_wr"][l].rearrange("(kc p) e -> p kc e", p=128), r=[], w=["wr_f"])
        self.dma(self.br_bc[:], d["moe_br_bc"][l], r=[], w=["br_bc"])
        for kc in range(KC):
            self.V(lambda e, kc=kc: e.tensor_scalar(out=self.wr_f[:, kc, :], in0=self.wr_f[:, kc, :],
                                                   scalar1=self.nfw[:, l * 16 + kc:l * 16 + kc + 1], scalar2=None,
                                                   op0=ALU.mult), r=["wr_f"], w=["wr_f"])
        self.V(lambda e: e.memset(self.carry_bc[:], 0.0), r=[], w=["carry_bc"])

    def defer(self, fn):
        pend = self.__dict__.setdefault("_pend", [])
        if pend:
            pend.pop()()
        pend.append(fn)

    def flush_defer(self):
        pend = self.__dict__.setdefault("_pend", [])
        while pend:
            pend.pop()()

    def outproj_chunk(self, l, j, dc, src, pbank):
        def load(dcx):
            self.dma(self.t_hch[dcx % 3], src[dcx * 128:(dcx + 1) * 128, j * TT:(j + 1) * TT], r=[],
                     w=[("hch", dcx % 3)])
        if dc == 0:
            load(0)
            load(1)
        if dc + 2 < KC:
            load(dc + 2)
        hb = self.t_hch[dc % 3]
        hk = ("hch", dc % 3)
        hm = self.t_hm[dc % 2]
        mk = ("hm", dc % 2)
        self.V(lambda e: e.tensor_tensor(out=hm, in0=self.pb(pbank)[:, :], in1=hb, op=ALU.add),
               r=[("ps", pbank), hk], w=[mk])
        self.dma(self.hmixT[dc * 128:(dc + 1) * 128, j * TT:(j + 1) * TT], hm, r=[mk], w=[("hmixT", j)])
        sq = self.t_sq[dc % 2]
        sk = ("sq", dc % 2)
        self.Sc(lambda e: e.activation(out=sq, in_=hm, func=AF.Square), r=[mk], w=[sk])
        xnb = self.t_xnb[dc % 2]
        xk = ("xnb", dc % 2)
        self.V(lambda e: e.tensor_scalar(out=xnb, in0=hm, scalar1=self.nfw[:, l * 16 + dc:l * 16 + dc + 1],
                                         scalar2=None, op0=ALU.mult), r=[mk], w=[xk])

        def part2():
            for i in range(4):
                self.mm(self.pb(7)[:, 288 + i:289 + i], sq[:, i * 128:(i + 1) * 128], self.ones_b[:, 0:1],
                        dc == 0 and i == 0, dc == KC - 1, r=[sk], w=[("ps", 7)])
                self.mm(self.pb(7)[:, i * 72:(i + 1) * 72], hm[:, i * 128:(i + 1) * 128], self.wr_f[:, dc, :],
                        False, dc == KC - 1, r=[mk, "wr_f"], w=[("ps", 7)])
            b = self.bank()
            pv = self.pbf(b)[:, 0:512].rearrange("p (i c) -> p i c", c=128)
            for i in range(4):
                self.tr(pv[:, i, :], xnb[:, i * 128:(i + 1) * 128], self.ident_b[:, :], r=[xk], w=[("ps", b)])
            self.Sc(lambda e: e.activation(out=self.t_rows[:, :, dc * 128:(dc + 1) * 128], in_=pv, func=AF.Copy),
                    r=[("ps", b)], w=[("rows", dc)])
        self.defer(part2)

    def moe_route(self, l, j):
        T = self.t_r
        V = self.V
        Sc = self.Sc
        self.flush_defer()
        V(lambda e: e.tensor_copy(out=T["ss2"], in_=self.pb(7)[:, 288:292]), r=[("ps", 7)], w=["ss2"])
        self.rstd_from(T["ss2"], "ss2", T["rs2"], "rs2", T["ss2"], "ss2", 1.0 / D)
        for i in range(4):
            V(lambda e, i=i: e.tensor_scalar(out=self.t_rows[:, i, :], in0=self.t_rows[:, i, :],
                                             scalar1=T["rs2"][:, i:i + 1], scalar2=None, op0=ALU.mult),
              r=["rows", "rs2"], w=["rows"])
        for i in range(4):
            ti = j * 4 + i
            V(lambda e, i=i: e.scalar_tensor_tensor(out=T["lg"], in0=self.pb(7)[:, i * 72:(i + 1) * 72],
                                                    scalar=T["rs2"][:, i:i + 1], in1=self.br_bc[:, :],
                                                    op0=ALU.mult, op1=ALU.add),
              r=[("ps", 7), "rs2", "br_bc"], w=["lg"])
            V(lambda e: e.tensor_reduce(out=T["gmax"], in_=T["lg"][:, 0:8], axis=AX.X, op=ALU.max),
              r=["lg"], w=["gmax"])
            V(lambda e: e.tensor_scalar(out=T["ngmax"], in0=T["gmax"], scalar1=-1.0, scalar2=None, op0=ALU.mult),
              r=["gmax"], w=["ngmax"])
            Sc(lambda e: e.activation(out=T["gex"], in_=T["lg"][:, 0:8], func=AF.Exp, bias=T["ngmax"][:, 0:1],
                                      accum_out=T["gsum"]), r=["lg", "ngmax"], w=["gex", "gsum"])
            V(lambda e: e.reciprocal(out=T["gw"], in_=T["gsum"]), r=["gsum"], w=["gw"])
            V(lambda e: e.tensor_scalar(out=T["goh"], in0=T["lg"][:, 0:8], scalar1=T["gmax"][:, 0:1], scalar2=None,
                                        op0=ALU.is_equal), r=["lg", "gmax"], w=["goh"])
            t88 = T["t88"].rearrange("p (j g) -> p j g", g=8)
            V(lambda e: e.tensor_tensor(out=t88, in0=T["lg"][:, 8:72].rearrange("p (g j) -> p j g", g=8),
                                        in1=T["goh"].unsqueeze(1).to_broadcast([128, 8, 8]), op=ALU.mult),
              r=["lg", "goh"], w=["t88"])
            V(lambda e: e.tensor_reduce(out=T["esel"], in_=t88, axis=AX.X, op=ALU.add), r=["t88"], w=["esel"])
            V(lambda e: e.max(out=T["top8"], in_=T["esel"]), r=["esel"], w=["top8"])
            V(lambda e: e.tensor_scalar(out=T["oh1"], in0=T["esel"], scalar1=T["top8"][:, 0:1], scalar2=None,
                                        op0=ALU.is_equal), r=["esel", "top8"], w=["oh1"])
            V(lambda e: e.tensor_scalar(out=T["oh2"], in0=T["esel"], scalar1=T["top8"][:, 1:2], scalar2=None,
                                        op0=ALU.is_equal), r=["esel", "top8"], w=["oh2"])
            V(lambda e: e.tensor_tensor(out=T["dd"], in0=T["top8"][:, 1:2], in1=T["top8"][:, 0:1], op=ALU.subtract),
              r=["top8"], w=["dd"])
            Sc(lambda e: e.activation(out=T["ex"], in_=T["dd"], func=AF.Exp), r=["dd"], w=["ex"])
            V(lambda e: e.tensor_scalar(out=T["ex1"], in0=T["ex"], scalar1=1.0, scalar2=None, op0=ALU.add),
              r=["ex"], w=["ex1"])
            V(lambda e: e.reciprocal(out=T["ew1"], in_=T["ex1"]), r=["ex1"], w=["ew1"])
            V(lambda e: e.tensor_tensor(out=T["ew2"], in0=T["ex"], in1=T["ew1"], op=ALU.mult),
              r=["ex", "ew1"], w=["ew2"])
            for k, (ohn, o64n, ewn) in enumerate([("oh1", "o641", "ew1"), ("oh2", "o642", "ew2")]):
                V(lambda e, ohn=ohn, o64n=o64n: e.tensor_tensor(
                    out=T[o64n].rearrange("p (g j) -> p g j", j=8),
                    in0=T["goh"].unsqueeze(2).to_broadcast([128, 8, 8]),
                    in1=T[ohn].unsqueeze(1).to_broadcast([128, 8, 8]), op=ALU.mult),
                  r=["goh", ohn], w=[o64n])
            V(lambda e: e.tensor_tensor(out=self.t_Ab, in0=T["o641"], in1=T["o642"], op=ALU.add),
              r=["o641", "o642"], w=["Ab"])
            b = self.bank()
            self.mm(self.pb(b)[:, 0:64], self.tris_b[:, :], self.t_Ab, True, True, r=["Ab"], w=[("ps", b)])
            self.mm(self.pb(b)[:, 64:128], self.ones_b[:, :], self.t_Ab, True, True, r=["Ab"], w=[("ps", b)])
            V(lambda e, b=b: e.tensor_tensor(out=T["cnt"], in0=self.pb(b)[:, 0:64], in1=self.carry_bc[:, :], op=ALU.add),
              r=[("ps", b), "carry_bc"], w=["cnt"])
            V(lambda e, b=b: e.tensor_tensor(out=self.carry_bc[:, :], in0=self.pb(b)[:, 64:128], in1=self.carry_bc[:, :],
                                             op=ALU.add), r=[("ps", b), "carry_bc"], w=["carry_bc"])
            for k, (o64n, ewn) in enumerate([("o641", "ew1"), ("o642", "ew2")]):
                V(lambda e, o64n=o64n: e.tensor_tensor(out=T["j64"], in0=T[o64n], in1=T["cnt"], op=ALU.mult),
                  r=[o64n, "cnt"], w=["j64"])
                V(lambda e: e.tensor_reduce(out=T["pos"], in_=T["j64"], axis=AX.X, op=ALU.add), r=["j64"], w=["pos"])
                V(lambda e, o64n=o64n: e.tensor_tensor(out=T["j64"], in0=T[o64n], in1=self.ebase[:, :], op=ALU.mult),
                  r=[o64n, "pos"], w=["j64"])
                V(lambda e: e.tensor_reduce(out=T["base"], in_=T["j64"], axis=AX.X, op=ALU.add), r=["j64"], w=["base"])
                V(lambda e: e.tensor_scalar(out=T["ovf"], in0=T["pos"], scalar1=float(CAP) - 0.5, scalar2=None,
                                            op0=ALU.is_gt), r=["pos"], w=["ovf"])
                V(lambda e: e.tensor_scalar(out=T["nov"], in0=T["ovf"], scalar1=-1.0, scalar2=1.0, op0=ALU.mult,
                                            op1=ALU.add), r=["ovf"], w=["nov"])
                V(lambda e: e.tensor_tensor(out=T["slf"], in0=T["pos"], in1=T["base"], op=ALU.add),
                  r=["pos", "base"], w=["slf"])
                V(lambda e: e.scalar_tensor_tensor(out=T["slf"], in0=T["ovf"], scalar=1.0e6, in1=T["slf"],
                                                   op0=ALU.mult, op1=ALU.add), r=["ovf", "slf"], w=["slf"])
                V(lambda e, k=k, ti=ti: e.tensor_copy(out=self.slots_i[:, ti, k:k + 1], in_=T["slf"]),
                  r=["slf"], w=[("slots", ti)])
                V(lambda e, k=k, ti=ti, ewn=ewn: e.scalar_tensor_tensor(
                    out=self.gates[:, ti, k:k + 1], in0=T[ewn], scalar=T["gw"][:, 0:1], in1=T["nov"],
                    op0=ALU.mult, op1=ALU.mult), r=[ewn, "gw", "nov"], w=[("gates", ti)])
                self.P.op("gpsimd", lambda e, i=i, k=k, ti=ti: e.indirect_dma_start(
                    out=self.xslots[:, :], out_offset=bass.IndirectOffsetOnAxis(ap=self.slots_i[:, ti, k:k + 1], axis=0),
                    in_=self.t_rows[:, i, :], in_offset=None, bounds_check=self.bcreg(e), oob_is_err=False),
                    r=["rows", ("slots", ti)], w=[("xs_sc", ti * 2 + k)], dma=True)

    def phase_A0(self):
        d = self.din
        A = self.A
        P = self.P
        V, Sc = self.V, self.Sc
        self.alloc_common()
        qT = A.alloc([128, 8, TT], BF16)
        kT = A.alloc([128, 8, TT], BF16)
        ktm = A.alloc([128, 4, 1024], BF16)
        vtm = A.alloc([128, 4, 2048], BF16)
        og = A.alloc([128, 4, 2048], BF16)
        hsT = self.t_hnT
        Cf = A.alloc([128, 4, 2, 512], F32)
        Cb = A.alloc([128, 4, 2, 512], BF16)
        nf = A.alloc([128, 8], F32)
        nb = A.alloc([128, 8], BF16)
        nwcol = A.alloc([128, 16], F32)
        wgate = A.alloc([128, KC, 8], BF16)
        junk = A.alloc([128, 512], BF16)
        PT = [A.alloc([128, 128], BF16) for _ in range(5)]
        kw = [A.alloc([128, 256], BF16) for _ in range(5)]
        G = {n: A.alloc([4, w], F32) for n, w in
             [("ig", 512), ("fg", 512), ("F", 512), ("M", 512), ("tw", 512), ("w", 512),
              ("w2", 512), ("z", 512), ("Mprev", 4), ("Mend", 4), ("dec", 4), ("Fc", 1), ("Mc", 1), ("bi", 1),
              ("bf", 1), ("bd", 16)]}
        G["t1"] = G["fg"]
        G["a"] = G["ig"]
        gcols = A.alloc([128, 48], F32)
        decbc = A.alloc([128, 16], F32)
        sm = {n: A.alloc([128, 1], F32) for n in ["ssq", "dn", "rden", "t", "rs", "scale"]}
        ones4 = A.alloc([4, 512], F32)
        zeros4 = A.alloc([4, 512], F32)
        V(lambda e: e.memset(ones4, 1.0), r=[], w=["ones4"])
        V(lambda e: e.memset(zeros4, 0.0), r=[], w=["zeros4"])
        self.t_ones4 = ones4
        self.arena_used_A0 = A.off

        self.dma(nwcol, d["ml_nw_col"], r=[], w=["nwcol"])
        P.op("gpsimd", lambda e: e.dma_start(
            out=wgate, in_=d["ml_w_in"][:, 6144:6152].rearrange("(kc p) e -> p kc e", p=128)), w=["wgate"], dma=True)
        self.dma(G["bi"], d["ml_bi"], r=[], w=["bi"])
        self.dma(G["bf"], d["ml_bf"], r=[], w=["bf"])
        V(lambda e: e.memset(Cf, 0.0), r=[], w=["Cf"])
        V(lambda e: e.memset(Cb, 0.0), r=[], w=["Cb"])
        V(lambda e: e.memset(nf, 0.0), r=[], w=["nf"])
        V(lambda e: e.memset(nb, 0.0), r=[], w=["nb"])
        V(lambda e: e.memset(G["Fc"], 0.0), r=[], w=["Fc"])
        V(lambda e: e.memset(G["Mc"], 0.0), r=[], w=["Mc"])
        self.moe_layer_setup(0)
        hnT, rstd_bc, rstd_col = self.t_hnT, self.t_rstd_bc, self.t_rstd_col
        Wi = d["ml_w_in"]
        ntiles = NT if self.stop not in ("A0t1",) else 1

        for j in range(ntiles):
            self.add_job(None, lambda wb, key, j=j: self.norm_in(d["xT"], j, self.nmw[:, 0:16], hnT, rstd_bc,
                                                                rstd_col, self.t_tmp_bc))
            for blk in range(8):
                def comp(wb, key, blk=blk):
                    wv = wb[:, 0:KC * 256].rearrange("p (kc e) -> p kc e", e=256)
                    for ec in range(2):
                        ch = blk * 2 + ec
                        b = self.bank()
                        for kc in range(KC):
                            self.mm(self.pb(b)[:, :], wv[:, kc, ec * 128:(ec + 1) * 128], hnT[:, kc, :], kc == 0,
                                    kc == KC - 1, r=[key, "hnT"], w=[("ps", b)])
                        if ch < 8:
                            V(lambda e, b=b, ch=ch: e.scalar_tensor_tensor(
                                out=qT[:, ch, :], in0=self.pb(b)[:, :], scalar=1.0 / 16.0, in1=rstd_bc,
                                op0=ALU.mult, op1=ALU.mult), r=[("ps", b), "rstd_bc"], w=["qT"])
                        else:
                            V(lambda e, b=b, ch=ch: e.tensor_tensor(out=kT[:, ch - 8, :], in0=self.pb(b)[:, :],
                                                                    in1=rstd_bc, op=ALU.mult),
                              r=[("ps", b), "rstd_bc"], w=["kT"])
                self.add_job(self.wload(Wi, 0, KC, blk * 256, 256), comp)
            for blk in range(20):
                def comp(wb, key, blk=blk):
                    wv = wb[:, 0:KC * 256].rearrange("p (kc e) -> p kc e", e=256)
                    for i in range(4):
                        b = self.bank()
                        for kc in range(KC):
                            self.mm(self.pb(b)[:, 0:256], hnT[:, kc, i * 128:(i + 1) * 128], wv[:, kc, :], kc == 0,
                                    kc == KC - 1, r=[key, "hnT"], w=[("ps", b)])
                        if blk < 4:
                            Sc(lambda e, b=b, i=i: e.activation(out=ktm[:, i, blk * 256:(blk + 1) * 256],
                                                                in_=self.pb(b)[:, 0:256], func=AF.Copy,
                                                                scale=rstd_col[:, i:i + 1]),
                               r=[("ps", b), "rstd_col"], w=["ktm"])
                        elif blk < 12:
                            c0 = (blk - 4) * 256
                            Sc(lambda e, b=b, i=i, c0=c0: e.activation(out=vtm[:, i, c0:c0 + 256],
                                                                       in_=self.pb(b)[:, 0:256], func=AF.Copy,
                                                                       scale=rstd_col[:, i:i + 1]),
                               r=[("ps", b), "rstd_col"], w=["vtm"])
                        else:
                            c0 = (blk - 12) * 256
                            Sc(lambda e, b=b, i=i, c0=c0: e.activation(out=og[:, i, c0:c0 + 256],
                                                                       in_=self.pb(b)[:, 0:256], func=AF.Sigmoid,
                                                                       scale=rstd_col[:, i:i + 1]),
                               r=[("ps", b), "rstd_col"], w=["og"])
                self.add_job(self.wload(Wi, 0, KC, 1024 + blk * 256, 256), comp)

            def rec(wb, key, j=j):
                bi_, bf_ = self.bank(), self.bank()
                for kc in range(KC):
                    self.mm(self.pb(bi_)[0:4, :], wgate[:, kc, 0:4], hnT[:, kc, :], kc == 0, kc == KC - 1,
                            r=["wgate", "hnT"], w=[("ps", bi_)])
                for kc in range(KC):
                    self.mm(self.pb(bf_)[0:4, :], wgate[:, kc, 4:8], hnT[:, kc, :], kc == 0, kc == KC - 1,
                            r=["wgate", "hnT"], w=[("ps", bf_)])
                V(lambda e: e.tensor_tensor(out=G["ig"], in0=self.pb(bi_)[0:4, :], in1=rstd_bc[0:4, :], op=ALU.mult),
                  r=[("ps", bi_), "rstd_bc"], w=["ig"])
                V(lambda e: e.tensor_scalar(out=G["ig"], in0=G["ig"], scalar1=G["bi"][:, 0:1], scalar2=None,
                                            op0=ALU.add), r=["ig", "bi"], w=["ig"])
                V(lambda e: e.tensor_tensor(out=G["fg"], in0=self.pb(bf_)[0:4, :], in1=rstd_bc[0:4, :], op=ALU.mult),
                  r=[("ps", bf_), "rstd_bc"], w=["fg"])
                V(lambda e: e.tensor_scalar(out=G["fg"], in0=G["fg"], scalar1=G["bf"][:, 0:1], scalar2=None,
                                            op0=ALU.add), r=["fg", "bf"], w=["fg"])
                Sc(lambda e: e.activation(out=G["t1"], in_=G["fg"], func=AF.Exp, scale=-1.0), r=["fg"], w=["fg"])
                Sc(lambda e: e.activation(out=G["t1"], in_=G["t1"], func=AF.Ln, bias=1.0), r=["fg"], w=["fg"])
                V(lambda e: e.tensor_tensor_scan(out=G["F"], data0=self.t_ones4, data1=G["t1"],
                                                 initial=G["Fc"][:, 0:1], op0=ALU.mult, op1=ALU.subtract),
                  r=["fg", "Fc", "ones4"], w=["F"])
                V(lambda e: e.tensor_copy(out=G["Fc"], in_=G["F"][:, 511:512]), r=["F"], w=["Fc"])
                V(lambda e: e.tensor_tensor(out=G["a"], in0=G["ig"], in1=G["F"], op=ALU.subtract),
                  r=["ig", "F"], w=["ig"])
                V(lambda e: e.tensor_tensor_scan(out=G["M"], data0=zeros4, data1=G["a"],
                                                 initial=G["Mc"][:, 0:1], op0=ALU.add, op1=ALU.max),
                  r=["ig", "Mc", "zeros4"], w=["M"])
                V(lambda e: e.tensor_copy(out=G["Mprev"][:, 0:1], in_=G["Mc"]), r=["Mc"], w=["Mprev"])
                V(lambda e: e.tensor_copy(out=G["Mprev"][:, 1:4], in_=G["M"][:, 127:384:128]), r=["M"], w=["Mprev"])
                V(lambda e: e.tensor_copy(out=G["Mend"], in_=G["M"][:, 127:512:128]), r=["M"], w=["Mend"])
                V(lambda e: e.tensor_copy(out=G["Mc"], in_=G["M"][:, 511:512]), r=["M"], w=["Mc"])
                v3 = lambda ap: ap.rearrange("p (c t) -> p c t", t=128)
                bc = lambda ap: ap.unsqueeze(2).to_broadcast([4, 4, 128])
                V(lambda e: e.tensor_tensor(out=v3(G["tw"]), in0=v3(G["a"]), in1=bc(G["Mprev"]), op=ALU.subtract),
                  r=["ig", "Mprev"], w=["tw"])
                Sc(lambda e: e.activation(out=G["w"], in_=G["tw"], func=AF.Exp), r=["tw"], w=["w"])
                V(lambda e: e.tensor_tensor(out=v3(G["tw"]), in0=v3(G["a"]), in1=bc(G["Mend"]), op=ALU.subtract),
                  r=["ig", "Mend", "w"], w=["tw"])
                Sc(lambda e: e.activation(out=G["w2"], in_=G["tw"], func=AF.Exp), r=["tw"], w=["w2"])
                V(lambda e: e.tensor_tensor(out=v3(G["tw"]), in0=v3(G["F"]), in1=bc(G["Mprev"]), op=ALU.add),
                  r=["F", "Mprev", "w2"], w=["tw"])
                Sc(lambda e: e.activation(out=G["z"], in_=G["tw"], func=AF.Exp, scale=-1.0), r=["tw"], w=["z"])
                V(lambda e: e.tensor_tensor(out=G["dec"], in0=G["Mprev"], in1=G["Mend"], op=ALU.subtract),
                  r=["Mprev", "Mend"], w=["dec"])
                Sc(lambda e: e.activation(out=G["dec"], in_=G["dec"], func=AF.Exp), r=["dec"], w=["dec"])
                bt = self.bank()
                for si, sn in enumerate(["w", "w2", "z"]):
                    for c in range(4):
                        o0 = (si * 4 + c) * 4
                        self.tr(self.pb(bt)[:, o0:o0 + 4], G[sn][0:4, c * 128:(c + 1) * 128], self.ident_f[0:4, 0:4],
                                r=[sn], w=[("ps", bt)])
                V(lambda e: e.tensor_copy(out=gcols, in_=self.pb(bt)[:, 0:48]), r=[("ps", bt)], w=["gcols"])
                V(lambda e: e.tensor_tensor(out=G["bd"].rearrange("p (h c) -> p h c", c=4),
                                            in0=self.ident_f[0:4, 0:4].unsqueeze(2).to_broadcast([4, 4, 4]),
                                            in1=G["dec"].unsqueeze(1).to_broadcast([4, 4, 4]), op=ALU.mult),
                  r=["dec"], w=["bd"])
                bd_ = self.bank()
                self.mm(self.pb(bd_)[:, 0:16], self.ones_f[0:4, :], G["bd"], True, True, r=["bd"], w=[("ps", bd_)])
                V(lambda e: e.tensor_copy(out=decbc, in_=self.pb(bd_)[:, 0:16]), r=[("ps", bd_)], w=["decbc"])

                def stA(c, h):
                    cs = slice(c * 128, (c + 1) * 128)
                    wcol = gcols[:, (0 * 4 + c) * 4 + h:(0 * 4 + c) * 4 + h + 1]
                    w2col = gcols[:, (1 * 4 + c) * 4 + h:(1 * 4 + c) * 4 + h + 1]
                    zcol = gcols[:, (2 * 4 + c) * 4 + h:(2 * 4 + c) * 4 + h + 1]
                    dcol = decbc[:, h * 4 + c:h * 4 + c + 1]
                    pi = (c * 4 + h) % 5
                    bs = self.bank()
                    for kk in range(2):
                        self.mm(self.pb(bs)[:, 0:128], kT[:, h * 2 + kk, cs], qT[:, h * 2 + kk, cs], kk == 0, kk == 1,
                                r=["kT", "qT"], w=[("ps", bs)])
                    V(lambda e, bs=bs, pi=pi, wcol=wcol: e.scalar_tensor_tensor(
                        out=PT[pi], in0=self.pb(bs)[:, 0:128], scalar=wcol, in1=self.tri_f[:, :],
                        op0=ALU.mult, op1=ALU.mult), r=[("ps", bs), "gcols"], w=[("PT", pi)])
                    V(lambda e, pi=pi, w2col=w2col, c=c, h=h: e.tensor_scalar(
                        out=kw[pi], in0=ktm[:, c, h * 256:(h + 1) * 256], scalar1=w2col, scalar2=None,
                        op0=ALU.mult), r=["ktm", "gcols"], w=[("kw", pi)])
                def stD(c, h):
                    cs = slice(c * 128, (c + 1) * 128)
                    wcol = gcols[:, (0 * 4 + c) * 4 + h:(0 * 4 + c) * 4 + h + 1]
                    w2col = gcols[:, (1 * 4 + c) * 4 + h:(1 * 4 + c) * 4 + h + 1]
                    zcol = gcols[:, (2 * 4 + c) * 4 + h:(2 * 4 + c) * 4 + h + 1]
                    dcol = decbc[:, h * 4 + c:h * 4 + c + 1]
                    pi = (c * 4 + h) % 5
                    bn = self.bank()
                    self.mm(self.pb(bn)[:, :], PT[pi], vtm[:, c, h * 512:(h + 1) * 512], True, False,
                            r=[("PT", pi), "vtm"], w=[("ps", bn)])
                    for kk in range(2):
                        self.mm(self.pb(bn)[:, :], qT[:, h * 2 + kk, cs], Cb[:, h, kk, :], False, kk == 1,
                                r=["qT", ("Cb", h)], w=[("ps", bn)])
                    bdn = self.bank()
                    self.mm(self.pb(bdn)[:, 0:1], PT[pi], self.ones_b[:, 0:1], True, False,
                            r=[("PT", pi)], w=[("ps", bdn)])
                    for kk in range(2):
                        self.mm(self.pb(bdn)[:, 0:1], qT[:, h * 2 + kk, cs], nb[:, h * 2 + kk:h * 2 + kk + 1],
                                False, kk == 1, r=["qT", ("nb", h)], w=[("ps", bdn)])
                    Sc(lambda e, bn=bn: e.activation(out=junk, in_=self.pb(bn)[:, :], func=AF.Square,
                                                     accum_out=sm["ssq"]), r=[("ps", bn)], w=["junk", "ssq"])
                    Sc(lambda e, bdn=bdn: e.activation(out=sm["dn"], in_=self.pb(bdn)[:, 0:1], func=AF.Abs),
                       r=[("ps", bdn)], w=["dn"])
                    V(lambda e, zcol=zcol: e.tensor_tensor(out=sm["dn"], in0=sm["dn"], in1=zcol, op=ALU.max),
                      r=["dn", "gcols"], w=["dn"])
                    V(lambda e: e.reciprocal(out=sm["rden"], in_=sm["dn"]), r=["dn"], w=["rden"])
                    V(lambda e: e.tensor_tensor(out=sm["t"], in0=sm["rden"], in1=sm["rden"], op=ALU.mult),
                      r=["rden"], w=["t"])
                    V(lambda e: e.tensor_tensor(out=sm["t"], in0=sm["t"], in1=sm["ssq"], op=ALU.mult),
                      r=["t", "ssq"], w=["t"])
                    self.rstd_from(sm["t"], "t", sm["rs"], "rs", sm["t"], "t", 1.0 / 512.0)
                    V(lambda e: e.tensor_tensor(out=sm["scale"], in0=sm["rs"], in1=sm["rden"], op=ALU.mult),
                      r=["rs", "rden"], w=["scale"])
                    V(lambda e, bn=bn, c=c, h=h: e.scalar_tensor_tensor(
                        out=og[:, c, h * 512:(h + 1) * 512], in0=self.pb(bn)[:, :], scalar=sm["scale"][:, 0:1],
                        in1=og[:, c, h * 512:(h + 1) * 512], op0=ALU.mult, op1=ALU.mult),
                      r=[("ps", bn), "scale", "og"], w=["og"])
                    bnn = self.bank()
                    for kk in range(2):
                        bc_ = self.bank()
                        self.mm(self.pb(bc_)[:, :], kw[pi][:, kk * 128:(kk + 1) * 128],
                                vtm[:, c, h * 512:(h + 1) * 512], True, True, r=[("kw", pi), "vtm"],
                                w=[("ps", bc_)])
                        self.mm(self.pb(bnn)[:, kk:kk + 1], kw[pi][:, kk * 128:(kk + 1) * 128],
                                self.ones_b[:, 0:1], True, True, r=[("kw", pi)], w=[("ps", bnn)])
                        V(lambda e, bc_=bc_, h=h, kk=kk, dcol=dcol: e.scalar_tensor_tensor(
                            out=Cf[:, h, kk, :], in0=Cf[:, h, kk, :], scalar=dcol, in1=self.pb(bc_)[:, :],
                            op0=ALU.mult, op1=ALU.add), r=[("ps", bc_), ("Cf", h), "decbc"], w=[("Cf", h)])
                        Sc(lambda e, h=h, kk=kk: e.activation(out=Cb[:, h, kk, :], in_=Cf[:, h, kk, :],
                                                              func=AF.Copy), r=[("Cf", h)], w=[("Cb", h)])
                    V(lambda e, bnn=bnn, h=h, dcol=dcol: e.scalar_tensor_tensor(
                        out=nf[:, h * 2:h * 2 + 2], in0=nf[:, h * 2:h * 2 + 2], scalar=dcol,
                        in1=self.pb(bnn)[:, 0:2], op0=ALU.mult, op1=ALU.add),
                      r=[("ps", bnn), ("nf", h), "decbc"], w=[("nf", h)])
                    V(lambda e, h=h: e.tensor_copy(out=nb[:, h * 2:h * 2 + 2], in_=nf[:, h * 2:h * 2 + 2]),
                      r=[("nf", h)], w=[("nb", h)])
                stp = [(c, h) for c in range(4) for h in range(4)]
                LA0 = 4
                for s_ in range(16 + LA0):
                    if s_ < 16:
                        stA(*stp[s_])
                    if s_ - LA0 >= 0:
                        stD(*stp[s_ - LA0])
                for ec in range(KC):
                    b = self.bank()
                    pv = self.pbf(b)[:, 0:512]
                    for i in range(4):
                        self.tr(pv[:, i * 128:(i + 1) * 128], og[:, i, ec * 128:(ec + 1) * 128], self.ident_b[:, :],
                                r=["og"], w=[("ps", b)])
                    Sc(lambda e, ec=ec, pv=pv: e.activation(out=hsT[:, ec, :], in_=pv, func=AF.Copy,
                                                            scale=nwcol[:, ec:ec + 1]),
                       r=[("ps", b), "nwcol"], w=["hnT"])
            self.add_job(None, rec)

            for blk in range(8):
                def comp(wb, key, blk=blk, j=j):
                    wv = wb[:, 0:KC * 256].rearrange("p (kc e) -> p kc e", e=256)
                    for ec in range(2):
                        dc = blk * 2 + ec
                        b = self.bank()
                        for kc in range(KC):
                            self.mm(self.pb(b)[:, :], wv[:, kc, ec * 128:(ec + 1) * 128], hsT[:, kc, :], kc == 0,
                                    kc == KC - 1, r=[key, "hnT"], w=[("ps", b)])
                        self.outproj_chunk(0, j, dc, d["xT"], b)
                    if blk == 7:
                        self.moe_route(0, j)
                self.add_job(self.wload(d["ml_w_out"], 0, KC, blk * 256, 256), comp)
        self.run_jobs()


    def phase_A1(self):
        d = self.din
        A = self.A
        P = self.P
        V, Sc = self.V, self.Sc
        self.alloc_common()
        hnT, rstd_bc, rstd_col = self.t_hnT, self.t_rstd_bc, self.t_rstd_col
        ynT = A.alloc([128, 32, TT], BF16)
        stf = A.alloc([128, 8, 512], F32)
        stb = A.alloc([128, 512], BF16)
        zs = A.alloc([128, 4, 512], BF16)
        xs = A.alloc([128, 4, 512], BF16)
        Btm = A.alloc([128, 4, 128], BF16)
        BT = A.alloc([128, TT], BF16)
        CT = A.alloc([128, TT], BF16)
        ccar = A.alloc([128, 48, 3], F32)
        cw = A.alloc([128, 48, 4], F32)
        cb = A.alloc([128, 48], F32)
        uext = [A.alloc([128, 515], F32) for _ in range(2)]
        accb = [A.alloc([128, TT], F32) for _ in range(2)]
        xsTc = [A.alloc([128, TT], BF16) for _ in range(3)]
        wdt = A.alloc([128, KC, 64], BF16)
        dtT = A.alloc([64, TT], F32)
        daT = A.alloc([64, TT], F32)
        cumT = A.alloc([64, TT], F32)
        rstm = A.alloc([64, TT], F32)
        alogc = A.alloc([64, 1], F32)
        acol = A.alloc([64, 1], F32)
        dtbc = A.alloc([64, 1], F32)
        rhsd = [A.alloc([64, 64], F32) for _ in range(2)]
        Tm = {n: A.alloc([128, 4, 64], F32) for n in ["dt", "cum", "ncum", "ecum", "cl", "ecl", "te", "tmp"]}
        dtb_bc = A.alloc([128, 64], F32)
        d_bc = A.alloc([128, 64], F32)
        nwcol = A.alloc([128, 32], F32)
        cbmm = [A.alloc([128, 128], F32) for _ in range(2)]
        dec = [A.alloc([128, 128], F32) for _ in range(8)]
        WT = [A.alloc([128, 128], BF16) for _ in range(8)]
        y1 = [A.alloc([128, 512], F32) for _ in range(2)]
        y2 = A.alloc([128, 512], F32)
        junk = A.alloc([128, 512], BF16)
        xw = [A.alloc([128, 512], BF16) for _ in range(2)]
        sm = {n: A.alloc([128, 1], F32) for n in ["ssq", "rs"]}
        Wi = d["ssd_w_in"]
        self.arena_used_A1 = A.off

        self.dma(cw, d["conv_w"], r=[], w=["cw"])
        self.dma(cb, d["conv_b"], r=[], w=["cb"])
        self.dma(rstm, d["c_reset"], r=[], w=["rstm"])
        self.dma(alogc, d["alog_col"], r=[], w=["alogc"])
        self.dma(dtbc, d["dtb_col"], r=[], w=["dtbc"])
        self.dma(dtb_bc, d["dtb_bc"], r=[], w=["dtb_bc"])
        self.dma(d_bc, d["d_bc"], r=[], w=["d_bc"])
        self.dma(nwcol, d["ssd_nw_col"], r=[], w=["nwcol"])
        P.op("gpsimd", lambda e: e.dma_start(
            out=wdt, in_=Wi[:, 10240:10304].rearrange("(kc p) e -> p kc e", p=128)), w=["wdt"], dma=True)
        Sc(lambda e: e.activation(out=acol, in_=alogc, func=AF.Exp), r=["alogc"], w=["acol"])
        V(lambda e: e.tensor_scalar(out=acol, in0=acol, scalar1=-1.0, scalar2=None, op0=ALU.mult), r=["acol"], w=["acol"])
        V(lambda e: e.memset(ccar, 0.0), r=[], w=["ccar"])
        V(lambda e: e.memset(stf, 0.0), r=[], w=["stf"])
        self.moe_layer_setup(1)
        ntiles = NT if self.stop not in ("A1t1", "A1only_t1") else 1
        hv = lambda ap: ap.rearrange("p (h q) -> p h q", q=64)

        def conv_chunk(q, b, out_bf, okey):
            u = uext[q % 2]
            uk = ("uext", q % 2)
            ac = accb[q % 2]
            ak = ("accb", q % 2)
            V(lambda e: e.tensor_tensor(out=u[:, 3:515], in0=self.pb(b)[:, :], in1=rstd_bc, op=ALU.mult),
              r=[("ps", b), "rstd_bc"], w=[uk])
            Sc(lambda e: e.activation(out=u[:, 0:3], in_=ccar[:, q, :], func=AF.Copy), r=[("ccar", q)], w=[uk])
            V(lambda e: e.tensor_scalar(out=ac, in0=u[:, 3:515], scalar1=cw[:, q, 3:4], scalar2=cb[:, q:q + 1],
                                        op0=ALU.mult, op1=ALU.add), r=[uk, "cw", "cb"], w=[ak])
            for k in (2, 1, 0):
                V(lambda e, k=k: e.scalar_tensor_tensor(out=ac, in0=u[:, k:k + 512], scalar=cw[:, q, k:k + 1], in1=ac,
                                                        op0=ALU.mult, op1=ALU.add), r=[uk, ak, "cw"], w=[ak])
            Sc(lambda e: e.activation(out=ccar[:, q, :], in_=u[:, 512:515], func=AF.Copy), r=[uk], w=[("ccar", q)])
            Sc(lambda e: e.activation(out=out_bf, in_=ac, func=AF.Silu), r=[ak], w=[okey])

        def softplus_inplace(t, key):
            Sc(lambda e: e.activation(out=t, in_=t, func=AF.Exp), r=[key], w=[key])
            Sc(lambda e: e.activation(out=t, in_=t, func=AF.Ln, bias=1.0), r=[key], w=[key])

        pend = []

        def defer(fn):
            if len(pend) >= 2:
                pend.pop(0)()
            pend.append(fn)

        def flush():
            while pend:
                pend.pop(0)()

        for j in range(ntiles):
            def pre(wb, key, j=j):
                self.norm_in(self.hcur, j, self.nmw[:, 16:32], hnT, rstd_bc, rstd_col, self.t_tmp_bc)
                b = self.bank()
                for kc in range(KC):
                    self.mm(self.pb(b)[0:64, :], wdt[:, kc, :], hnT[:, kc, :], kc == 0, kc == KC - 1,
                            r=["wdt", "hnT"], w=[("ps", b)])
                V(lambda e: e.tensor_tensor(out=dtT, in0=self.pb(b)[0:64, :], in1=rstd_bc[0:64, :], op=ALU.mult),
                  r=[("ps", b), "rstd_bc"], w=["dtT"])
                V(lambda e: e.tensor_scalar(out=dtT, in0=dtT, scalar1=dtbc[:, 0:1], scalar2=None, op0=ALU.add),
                  r=["dtT", "dtbc"], w=["dtT"])
                softplus_inplace(dtT, "dtT")
                V(lambda e: e.tensor_scalar(out=daT, in0=dtT, scalar1=acol[:, 0:1], scalar2=None, op0=ALU.mult),
                  r=["dtT", "acol"], w=["daT"])
                V(lambda e: e.tensor_tensor_scan(out=cumT, data0=rstm, data1=daT, initial=0.0, op0=ALU.mult,
                                                 op1=ALU.add), r=["daT", "rstm"], w=["cumT"])
                for i in range(4):
                    b2 = self.bank()
                    for kc in range(KC):
                        self.mm(self.pb(b2)[:, 0:64], hnT[:, kc, i * 128:(i + 1) * 128], wdt[:, kc, :], kc == 0,
                                kc == KC - 1, r=["wdt", "hnT"], w=[("ps", b2)])
                    V(lambda e, i=i, b2=b2: e.scalar_tensor_tensor(out=Tm["dt"][:, i, :], in0=self.pb(b2)[:, 0:64],
                                                                   scalar=rstd_col[:, i:i + 1], in1=dtb_bc,
                                                                   op0=ALU.mult, op1=ALU.add),
                      r=[("ps", b2), "rstd_col", "dtb_bc"], w=["Tdt"])
                softplus_inplace(Tm["dt"], "Tdt")
                b3 = self.bank()
                for c in range(4):
                    self.tr(self.pb(b3)[:, c * 64:(c + 1) * 64], cumT[0:64, c * 128:(c + 1) * 128],
                            self.ident_f[0:64, 0:64], r=["cumT"], w=[("ps", b3)])
                V(lambda e: e.tensor_copy(out=Tm["cum"], in_=self.pb(b3)[:, 0:256].rearrange("p (c h) -> p c h", h=64)),
                  r=[("ps", b3)], w=["Tcum"])
                V(lambda e: e.tensor_scalar(out=Tm["ncum"], in0=Tm["cum"], scalar1=-1.0, scalar2=None, op0=ALU.mult),
                  r=["Tcum"], w=["Tncum"])
                Sc(lambda e: e.activation(out=Tm["ecum"], in_=Tm["cum"], func=AF.Exp), r=["Tcum"], w=["Tecum"])
                b4 = self.bank()
                for c in range(4):
                    rd = rhsd[c % 2]
                    rk = ("rhsd", c % 2)
                    V(lambda e, c=c, rd=rd: e.tensor_scalar(out=rd, in0=self.ident_f[0:64, 0:64],
                                                            scalar1=cumT[:, c * 128 + 127:c * 128 + 128], scalar2=None,
                                                            op0=ALU.mult), r=["cumT"], w=[rk])
                    self.mm(self.pb(b4)[:, c * 64:(c + 1) * 64], self.ones_f[0:64, :], rd, True, True, r=[rk],
                            w=[("ps", b4)])
                V(lambda e: e.tensor_copy(out=Tm["cl"], in_=self.pb(b4)[:, 0:256].rearrange("p (c h) -> p c h", h=64)),
                  r=[("ps", b4)], w=["Tcl"])
                Sc(lambda e: e.activation(out=Tm["ecl"], in_=Tm["cl"], func=AF.Exp), r=["Tcl"], w=["Tecl"])
                V(lambda e: e.tensor_tensor(out=Tm["tmp"], in0=Tm["cl"], in1=Tm["cum"], op=ALU.subtract),
                  r=["Tcl", "Tcum"], w=["Ttmp"])
                Sc(lambda e: e.activation(out=Tm["tmp"], in_=Tm["tmp"], func=AF.Exp), r=["Ttmp"], w=["Ttmp"])
                V(lambda e: e.tensor_tensor(out=Tm["te"], in0=Tm["tmp"], in1=Tm["dt"], op=ALU.mult),
                  r=["Ttmp", "Tdt"], w=["Tte"])
            self.add_job(None, pre)

            for g in range(8):
                for blk in range(2):
                    def comp(wb, key, blk=blk, g=g):
                        wv = wb[:, 0:KC * 256].rearrange("p (kc e) -> p kc e", e=256)
                        for i in range(4):
                            b = self.bank()
                            for kc in range(KC):
                                self.mm(self.pb(b)[:, 0:256], hnT[:, kc, i * 128:(i + 1) * 128], wv[:, kc, :], kc == 0,
                                        kc == KC - 1, r=[key, "hnT"], w=[("ps", b)])
                            Sc(lambda e, b=b, i=i: e.activation(out=zs[:, i, blk * 256:(blk + 1) * 256],
                                                                in_=self.pb(b)[:, 0:256], func=AF.Silu,
                                                                scale=rstd_col[:, i:i + 1]),
                               r=[("ps", b), "rstd_col"], w=["zs"])
                    self.add_job(self.wload(Wi, 0, KC, g * 512 + blk * 256, 256), comp)
                for blk in range(2):
                    def comp(wb, key, blk=blk, g=g):
                        wv = wb[:, 0:KC * 256].rearrange("p (kc e) -> p kc e", e=256)
                        for ec in range(2):
                            cc = blk * 2 + ec
                            q = g * 4 + cc
                            b = self.bank()
                            for kc in range(KC):
                                self.mm(self.pb(b)[:, :], wv[:, kc, ec * 128:(ec + 1) * 128], hnT[:, kc, :], kc == 0,
                                        kc == KC - 1, r=[key, "hnT"], w=[("ps", b)])
                            xo = xsTc[q % 3]
                            xk = ("xsTc", q % 3)
                            conv_chunk(q, b, xo, xk)

                            def trx(xo=xo, xk=xk, cc=cc):
                                b2 = self.bank()
                                pv = self.pbf(b2)[:, 0:512].rearrange("p (i c) -> p i c", c=128)
                                for i in range(4):
                                    self.tr(pv[:, i, :], xo[:, i * 128:(i + 1) * 128], self.ident_b[:, :], r=[xk],
                                            w=[("ps", b2)])
                                V(lambda e, pv=pv, cc=cc: e.tensor_copy(out=xs[:, :, cc * 128:(cc + 1) * 128], in_=pv),
                                  r=[("ps", b2)], w=["xs"])
                            defer(trx)
                    self.add_job(self.wload(Wi, 0, KC, 4096 + g * 512 + blk * 256, 256), comp)
                for which in range(2):
                    def comp(wb, key, which=which, g=g):
                        wv = wb[:, 0:KC * 128].rearrange("p (kc e) -> p kc e", e=128)
                        q = 32 + which * 8 + g
                        b = self.bank()
                        for kc in range(KC):
                            self.mm(self.pb(b)[:, :], wv[:, kc, :], hnT[:, kc, :], kc == 0, kc == KC - 1,
                                    r=[key, "hnT"], w=[("ps", b)])
                        if which == 0:
                            conv_chunk(q, b, BT, "BT")

                            def trb():
                                b2 = self.bank()
                                pv = self.pbf(b2)[:, 0:512].rearrange("p (i c) -> p i c", c=128)
                                for i in range(4):
                                    self.tr(pv[:, i, :], BT[:, i * 128:(i + 1) * 128], self.ident_b[:, :], r=["BT"],
                                            w=[("ps", b2)])
                                V(lambda e, pv=pv: e.tensor_copy(out=Btm, in_=pv), r=[("ps", b2)], w=["Btm"])
                            defer(trb)
                        else:
                            conv_chunk(q, b, CT, "CT")
                    self.add_job(self.wload(Wi, 0, KC, 8192 + which * 1024 + g * 128, 128), comp)

                def rec(wb, key, g=g, j=j):
                    gs = slice(g * 8, (g + 1) * 8)
                    bc8 = lambda ap: ap.unsqueeze(2).to_broadcast([128, 8, 64])
                    flush()
                    Sc(lambda e: e.activation(out=stb, in_=stf[:, g, :], func=AF.Copy), r=[("stf", g)], w=["stb"])
                    LA = 6
                    steps = [(c, hl) for c in range(4) for hl in range(8)]

                    def cbm_pre(c):
                        cs = slice(c * 128, (c + 1) * 128)
                        pi = c % 2
                        b1 = self.bank()
                        self.mm(self.pb(b1)[:, 0:128], BT[:, cs], CT[:, cs], True, True, r=["BT", "CT"], w=[("ps", b1)])
                        V(lambda e, b1=b1, pi=pi: e.tensor_tensor(out=cbmm[pi], in0=self.pb(b1)[:, 0:128],
                                                                  in1=self.tri_f[:, :], op=ALU.mult),
                          r=[("ps", b1)], w=[("cbmm", pi)])

                    def yoff(c):
                        cs = slice(c * 128, (c + 1) * 128)
                        ya = y1[c % 2]
                        yk = ("y1", c % 2)
                        b2 = self.bank()
                        self.mm(self.pb(b2)[:, :], CT[:, cs], stb, True, True, r=["CT", "stb"], w=[("ps", b2)])
                        V(lambda e, b2=b2, ya=ya, c=c: e.tensor_tensor(out=hv(ya), in0=hv(self.pb(b2)[:, :]),
                                                                       in1=bc8(Tm["ecum"][:, c, gs]), op=ALU.mult),
                          r=[("ps", b2), "Tecum"], w=[yk])

                    def stageA(s_):
                        c, hl = steps[s_]
                        cs = slice(c * 128, (c + 1) * 128)
                        h = g * 8 + hl
                        p2 = s_ % 8
                        pi = c % 2
                        bE = self.bank()
                        self.mm(self.pb(bE)[:, 0:128], self.ident_f[0:64, h:h + 1].to_broadcast([64, 128]),
                                cumT[0:64, cs], True, False, r=["cumT"], w=[("ps", bE)])
                        self.mm(self.pb(bE)[:, 0:128], self.ident_b[:, :], self.neg_b[:, :], False, True, r=[],
                                w=[("ps", bE)])
                        Sc(lambda e, bE=bE, p2=p2, c=c, h=h: e.activation(
                            out=dec[p2], in_=self.pb(bE)[:, 0:128], func=AF.Exp, bias=Tm["ncum"][:, c, h:h + 1]),
                           r=[("ps", bE), "Tncum"], w=[("dec", p2)])
                        V(lambda e, p2=p2, pi=pi, c=c, h=h: e.scalar_tensor_tensor(
                            out=WT[p2], in0=dec[p2], scalar=Tm["dt"][:, c, h:h + 1], in1=cbmm[pi], op0=ALU.mult,
                            op1=ALU.mult), r=[("dec", p2), ("cbmm", pi), "Tdt"], w=[("WT", p2)])

                    def stageD(s_):
                        c, hl = steps[s_]
                        p2 = s_ % 8
                        self.mm(self.pb(6 + c % 2)[:, hl * 64:(hl + 1) * 64], WT[p2], xs[:, c, hl * 64:(hl + 1) * 64],
                                True, True, r=[("WT", p2), "xs"], w=[("ps", 6 + c % 2)])

                    def st_pre(c):
                        pi = c % 2
                        V(lambda e, pi=pi, c=c: e.tensor_tensor(out=hv(xw[pi]), in0=hv(xs[:, c, :]),
                                                                in1=bc8(Tm["te"][:, c, gs]), op=ALU.mult),
                          r=["xs", "Tte"], w=[("xw", pi)])

                    def st_mm(c):
                        pi = c % 2
                        b4 = self.bank()
                        self.mm(self.pb(b4)[:, :], Btm[:, c, :], xw[pi], True, True, r=["Btm", ("xw", pi)],
                                w=[("ps", b4)])
                        V(lambda e, c=c: e.tensor_tensor(out=hv(stf[:, g, :]), in0=hv(stf[:, g, :]),
                                                         in1=bc8(Tm["ecl"][:, c, gs]), op=ALU.mult),
                          r=[("stf", g), "Tecl"], w=[("stf", g)])
                        V(lambda e, b4=b4: e.tensor_tensor(out=stf[:, g, :], in0=stf[:, g, :], in1=self.pb(b4)[:, :],
                                                           op=ALU.add), r=[("stf", g), ("ps", b4)], w=[("stf", g)])
                        if c < 3:
                            Sc(lambda e: e.activation(out=stb, in_=stf[:, g, :], func=AF.Copy), r=[("stf", g)],
                               w=["stb"])

                    def post_y(c):
                        pi = c % 2
                        ya = y1[pi]
                        yk = ("y1", pi)
                        bd = 6 + c % 2
                        V(lambda e, ya=ya: e.tensor_tensor(out=ya, in0=ya, in1=self.pb(bd)[:, :], op=ALU.add),
                          r=[("ps", bd), yk], w=[yk])
                        V(lambda e, c=c: e.tensor_tensor(out=hv(y2), in0=hv(xs[:, c, :]), in1=bc8(d_bc[:, gs]),
                                                         op=ALU.mult), r=["xs", "d_bc"], w=["y2"])
                        V(lambda e, ya=ya: e.tensor_tensor(out=ya, in0=ya, in1=y2, op=ALU.add), r=[yk, "y2"], w=[yk])
                        V(lambda e, ya=ya, c=c: e.tensor_tensor(out=ya, in0=ya, in1=zs[:, c, :], op=ALU.mult),
                          r=[yk, "zs"], w=[yk])
                        Sc(lambda e, ya=ya: e.activation(out=junk, in_=ya, func=AF.Square, accum_out=sm["ssq"]),
                           r=[yk], w=["junk", "ssq"])
                        self.rstd_from(sm["ssq"], "ssq", sm["rs"], "rs", sm["ssq"], "ssq", 1.0 / 512.0)
                        V(lambda e, ya=ya, c=c: e.tensor_scalar(out=zs[:, c, :], in0=ya, scalar1=sm["rs"][:, 0:1],
                                                                scalar2=None, op0=ALU.mult), r=[yk, "rs"], w=["zs"])

                    cbm_pre(0)
                    yoff(0)
                    for s_ in range(32 + LA):
                        if s_ < 32:
                            c, hl = steps[s_]
                            if hl == 0 and c + 1 < 4:
                                cbm_pre(c + 1)
                            stageA(s_)
                        sd = s_ - LA
                        if sd >= 0:
                            stageD(sd)
                            c, hl = steps[sd]
                            if hl == 7:
                                st_pre(c)
                                post_y(c)
                            if hl == 1 and c >= 1:
                                st_mm(c - 1)
                            if hl == 5 and c >= 1:
                                yoff(c)
                    st_mm(3)
                    for cc in range(4):
                        b = self.bank()
                        pv = self.pbf(b)[:, 0:512]
                        for i in range(4):
                            self.tr(pv[:, i * 128:(i + 1) * 128], zs[:, i, cc * 128:(cc + 1) * 128], self.ident_b[:, :],
                                    r=["zs"], w=[("ps", b)])
                        ec = g * 4 + cc
                        Sc(lambda e, pv=pv, ec=ec: e.activation(out=ynT[:, ec, :], in_=pv, func=AF.Copy,
                                                                scale=nwcol[:, ec:ec + 1]),
                           r=[("ps", b), "nwcol"], w=[("ynT", ec)])
                self.add_job(None, rec)

            if self.dbg and j == 0 and self.stop == "A1only_t1":
                def dump(wb, key):
                    for nm, t, shp, dt_ in [("dtT", dtT, [64, TT], F32), ("cumT", cumT, [64, TT], F32),
                                            ("Tdt", Tm["dt"], [128, 4, 64], F32), ("Tcum", Tm["cum"], [128, 4, 64], F32),
                                            ("Tcl", Tm["cl"], [128, 4, 64], F32), ("Tte", Tm["te"], [128, 4, 64], F32),
                                            ("xs", xs, [128, 4, 512], BF16), ("BT", BT, [128, TT], BF16),
                                            ("CT", CT, [128, TT], BF16), ("Btm", Btm, [128, 4, 128], BF16),
                                            ("zs", zs, [128, 4, 512], BF16), ("ynT", ynT, [128, 32, TT], BF16),
                                            ("stf", stf, [128, 8, 512], F32), ("hnT", hnT, [128, KC, TT], BF16),
                                            ("rstd_bc", rstd_bc, [128, TT], F32), ("dec0", dec[0], [128, 128], F32),
                                            ("dec1", dec[1], [128, 128], F32), ("cbmm1", cbmm[1], [128, 128], F32),
                                            ("WT1", WT[1], [128, 128], BF16), ("y11", y1[1], [128, 512], F32),
                                            ("y2", y2, [128, 512], F32), ("Tncum", Tm["ncum"], [128, 4, 64], F32)]:
                        o = self.dbg_tensor("dbg_" + nm, shp, dt_)
                        self.dma(o, t, r=["cumT", "dtT", "Tdt", "Tcum", "Tcl", "Tte", "xs", "BT", "CT", "Btm", "zs",
                                          "ynT", "stf", "hnT", "rstd_bc", "dec", "cbmm", "WT", "y1", "y2", "Tncum"], w=[("dbgw", nm)])
                self.add_job(None, dump)
            for dc in range(KC):
                def comp(wb, key, dc=dc, j=j):
                    wv = wb[:, 0:32 * 128].rearrange("p (kc e) -> p kc e", e=128)
                    b = self.bank()
                    for ec in range(32):
                        self.mm(self.pb(b)[:, :], wv[:, ec, :], ynT[:, ec, :], ec == 0, ec == 31, r=[key, "ynT"],
                                w=[("ps", b)])
                    self.outproj_chunk(1, j, dc, self.hcur, b)
                    if dc == KC - 1:
                        self.moe_route(1, j)
                self.add_job(self.wload(d["ssd_w_out"], 0, 32, dc * 128, 128), comp)
        self.run_jobs()

    def phase_B(self, l):
        d = self.din
        A = self.A
        V, Sc = self.V, self.Sc
        xr = [A.alloc([128, 3, D], BF16) for _ in range(2)]
        xbT = A.alloc([128, KC, CAP], BF16)
        sg = A.alloc([128, 4, CAP], F32)
        hidT = A.alloc([128, 4, CAP], BF16)
        yrow = [A.alloc([128, D], F32) for _ in range(3)]
        sc_keys = [("xs_sc", i) for i in range(64)]
        extra = [A.alloc([128, 4096], BF16) for _ in range(10)]
        ne = NE if self.stop not in ("B0e2",) else 2
        for e_ in range(ne):
            xi = e_ % 2

            def load_x(en):
                for (r0, rn) in RB:
                    rb = r0 // 128
                    self.dma(xr[en % 2][0:rn, rb, :], self.xslots[en * CAP + r0:en * CAP + r0 + rn, :],
                             r=sc_keys, w=[("xr", en % 2)])

            def prep(wb, key, e_=e_, xi=xi):
                if e_ == 0:
                    load_x(0)
                if e_ + 1 < ne:
                    load_x(e_ + 1)
                for q4 in range(4):
                    for (r0, rn) in RB:
                        rb = r0 // 128
                        b = self.bank()
                        pv = self.pbf(b)[:, 0:512].rearrange("p (a r) -> p a r", r=128)
                        for a in range(4):
                            dc = q4 * 4 + a
                            self.tr(pv[:, a, 0:rn], xr[xi][0:rn, rb, dc * 128:(dc + 1) * 128], self.ident_b[0:rn, 0:rn],
                                    r=[("xr", xi)], w=[("ps", b)])
                        eng = Sc if (q4 + rb) % 2 == 0 else V
                        if eng is Sc:
                            Sc(lambda e, pv=pv, q4=q4, r0=r0, rn=rn: e.activation(
                                out=xbT[:, q4 * 4:(q4 + 1) * 4, r0:r0 + rn], in_=pv[:, :, 0:rn], func=AF.Copy),
                               r=[("ps", b)], w=["xbT"])
                        else:
                            V(lambda e, pv=pv, q4=q4, r0=r0, rn=rn: e.tensor_copy(
                                out=xbT[:, q4 * 4:(q4 + 1) * 4, r0:r0 + rn], in_=pv[:, :, 0:rn]),
                              r=[("ps", b)], w=["xbT"])
            self.add_job(None, prep)
            for which in range(2):
                W = d["moe_wg"] if which == 0 else d["moe_wu"]
                for blk in range(2):
                    def comp(wb, key, blk=blk, which=which):
                        wv = wb[:, 0:KC * 256].rearrange("p (kc e) -> p kc e", e=256)
                        for fc in range(2):
                            m = blk * 2 + fc
                            b = self.bank()
                            for kc in range(KC):
                                self.mm(self.pb(b)[:, 0:CAP], wv[:, kc, fc * 128:(fc + 1) * 128], xbT[:, kc, :],
                                        kc == 0, kc == KC - 1, r=[key, "xbT"], w=[("ps", b)])
                            if which == 0:
                                Sc(lambda e, b=b, m=m: e.activation(out=sg[:, m, :], in_=self.pb(b)[:, 0:CAP],
                                                                    func=AF.Silu), r=[("ps", b)], w=[("sg", m)])
                            else:
                                V(lambda e, b=b, m=m: e.tensor_tensor(out=hidT[:, m, :], in0=self.pb(b)[:, 0:CAP],
                                                                      in1=sg[:, m, :], op=ALU.mult),
                                  r=[("ps", b), ("sg", m)], w=[("hidT", m)])
                    self.add_job(self.wload(W[l, e_], 0, KC, blk * 256, 256), comp)
            for blk in range(2):
                def comp(wb, key, blk=blk, e_=e_):
                    wv = wb[:, 0:4 * 1024].rearrange("p (kc e) -> p kc e", e=1024)
                    for (r0, rn) in RB:
                        rb = r0 // 128
                        for nb_ in range(2):
                            b = self.bank()
                            for m in range(4):
                                self.mm(self.pb(b)[0:rn, :], hidT[:, m, r0:r0 + rn], wv[:, m, nb_ * 512:(nb_ + 1) * 512],
                                        m == 0, m == 3, r=[key, "hidT"], w=[("ps", b)])
                            c0 = blk * 1024 + nb_ * 512
                            if nb_ == 0:
                                Sc(lambda e, b=b, rb=rb, rn=rn, c0=c0: e.activation(
                                    out=yrow[rb][0:rn, c0:c0 + 512], in_=self.pb(b)[0:rn, :], func=AF.Copy),
                                   r=[("ps", b)], w=[("yrow", rb)])
                            else:
                                V(lambda e, b=b, rb=rb, rn=rn, c0=c0: e.tensor_copy(
                                    out=yrow[rb][0:rn, c0:c0 + 512], in_=self.pb(b)[0:rn, :]),
                                  r=[("ps", b)], w=[("yrow", rb)])
                        if blk == 1:
                            self.dma(self.yslots[e_ * CAP + r0:e_ * CAP + r0 + rn, :], yrow[rb][0:rn, :],
                                     r=[("yrow", rb)], w=[("ys", e_)])
                self.add_job(self.wload(d["moe_wd"][l, e_], 0, 4, blk * 1024, 1024), comp)
        self.run_jobs(extra=extra)

    def phase_C(self, l):
        A = self.A
        V, Sc = self.V, self.Sc
        y = [[A.alloc([128, D], F32) for _ in range(2)] for _ in range(2)]
        moe = [A.alloc([128, D], F32) for _ in range(2)]
        hmb = [A.alloc([128, KC, 128], F32) for _ in range(2)]
        hn = [A.alloc([128, KC, 128], F32) for _ in range(2)]
        sq = [A.alloc([128, KC, 128], BF16) for _ in range(2)]
        rs = A.alloc([128, 128], F32)
        ys_keys = [("ys", e) for e in range(NE)]
        for p_ in range(2):
            for k in range(2):
                V(lambda e, p_=p_, k=k: e.memset(y[p_][k], 0.0), r=[], w=[("y", p_, k)])
        last = (l == 1)
        dst = self.outT if last else self.hcur
        nsub = 32 if self.stop not in ("C0s2",) else 2
        for ti in range(nsub):
            p_ = ti % 2
            ts = slice(ti * 128, (ti + 1) * 128)
            for k in range(2):
                self.P.op("gpsimd", lambda e, p_=p_, k=k, ti=ti: e.indirect_dma_start(
                    out=y[p_][k][:, :], out_offset=None, in_=self.yslots[:, :],
                    in_offset=bass.IndirectOffsetOnAxis(ap=self.slots_i[:, ti, k:k + 1], axis=0),
                    bounds_check=self.bcreg(e), oob_is_err=False), r=ys_keys, w=[("y", p_, k)], dma=True)
            self.dma(hmb[p_], self.hmixT[:, ts].rearrange("(kc p) t -> p kc t", p=128), r=[("hmixT", ti // 4)],
                     w=[("hmb", p_)])
            V(lambda e, p_=p_, ti=ti: e.tensor_scalar(out=moe[p_], in0=y[p_][0], scalar1=self.gates[:, ti, 0:1],
                                                     scalar2=None, op0=ALU.mult), r=[("y", p_, 0)], w=[("moe", p_)])
            V(lambda e, p_=p_, ti=ti: e.scalar_tensor_tensor(out=moe[p_], in0=y[p_][1], scalar=self.gates[:, ti, 1:2],
                                                            in1=moe[p_], op0=ALU.mult, op1=ALU.add),
              r=[("y", p_, 1), ("moe", p_)], w=[("moe", p_)])
            for q4 in range(4):
                b = self.bank()
                pv = self.pb(b)[:, :].rearrange("p (a t) -> p a t", t=128)
                for a in range(4):
                    dc = q4 * 4 + a
                    self.tr(pv[:, a, :], moe[p_][:, dc * 128:(dc + 1) * 128], self.ident_f[:, :],
                            r=[("moe", p_)], w=[("ps", b)])
                V(lambda e, pv=pv, q4=q4, p_=p_: e.tensor_tensor(out=hn[p_][:, q4 * 4:(q4 + 1) * 4, :], in0=pv,
                                                                in1=hmb[p_][:, q4 * 4:(q4 + 1) * 4, :], op=ALU.add),
                  r=[("ps", b), ("hmb", p_)], w=[("hn", p_)])
            if last:
                Sc(lambda e, p_=p_: e.activation(out=sq[p_], in_=hn[p_], func=AF.Square), r=[("hn", p_)],
                   w=[("sqc", p_)])
                b = self.bank()
                for kc in range(KC):
                    self.mm(self.pb(b)[:, 0:128], self.ones_b[:, :], sq[p_][:, kc, :], kc == 0, kc == KC - 1,
                            r=[("sqc", p_)], w=[("ps", b)])
                self.rstd_from(self.pb(b)[:, 0:128], ("ps", b), rs, "rsC", rs, "rsC", 1.0 / D)
                for kc in range(KC):
                    V(lambda e, kc=kc, p_=p_: e.scalar_tensor_tensor(
                        out=hn[p_][:, kc, :], in0=hn[p_][:, kc, :], scalar=self.fnw[:, kc:kc + 1], in1=rs,
                        op0=ALU.mult, op1=ALU.mult), r=[("hn", p_), "rsC"], w=[("hn", p_)])
            self.dma(dst[:, ts].rearrange("(kc p) t -> p kc t", p=128), hn[p_], r=[("hn", p_)], w=[("dst", ti // 4)])

    def build(self):
        stop = self.stop
        self.phase_barrier()
        if stop.startswith("A1only"):
            self.phase_A1()
            return self.finish()
        self.phase_A0()
        if stop.startswith("A0"):
            return self.finish()
        self.phase_barrier()
        self.phase_B(0)
        if stop.startswith("B0"):
            return self.finish()
        self.phase_barrier()
        self.phase_C(0)
        if stop.startswith("C0"):
            return self.finish()
        self.phase_barrier()
        self.phase_A1()
        if stop.startswith("A1"):
            return self.finish()
        self.phase_barrier()
        self.phase_B(1)
        self.phase_barrier()
        self.phase_C(1)
        return self.finish()

    def finish(self):
        if self.dbg:
            self.dma(self.dbg_slots, self.slots_i[:].rearrange("p a b -> p (a b)"), r=["slots"], w=["dbg1"])
            self.dma(self.dbg_gates, self.gates[:].rearrange("p a b -> p (a b)"), r=["gates"], w=["dbg2"])
        self.P.emit()
        return self.nc


def _col(v, n):
    return np.ascontiguousarray(np.asarray(v, np.float32).reshape(n, 128).T)


def _shared_inputs(inp):
    f = lambda k: np.asarray(inp[k], np.float32)
    sh = {}
    sh["nmw"] = np.ascontiguousarray(np.concatenate([_col(f("norm_mix_w")[0], 16), _col(f("norm_mix_w")[1], 16)], axis=1))
    sh["nfw"] = np.ascontiguousarray(np.concatenate([_col(f("norm_ffn_w")[0], 16), _col(f("norm_ffn_w")[1], 16)], axis=1))
    sh["fnw"] = _col(f("final_norm_w"), 16)
    sh["ml_w_in"] = np.ascontiguousarray(f("ml_w_in")[0])
    sh["ml_bi"] = np.ascontiguousarray(f("ml_b_i")[0].reshape(4, 1))
    sh["ml_bf"] = np.ascontiguousarray(f("ml_b_f")[0].reshape(4, 1))
    sh["ml_nw_col"] = _col(f("ml_norm_w")[0], 16)
    sh["ml_w_out"] = np.ascontiguousarray(f("ml_w_out")[0])
    sh["ssd_w_in"] = np.ascontiguousarray(f("ssd_w_in")[0])
    sh["conv_w"] = np.ascontiguousarray(f("ssd_conv_w")[0].T.reshape(48, 128, 4).transpose(1, 0, 2))
    sh["conv_b"] = _col(f("ssd_conv_b")[0], 48)
    sh["dtb_col"] = np.ascontiguousarray(f("ssd_dt_bias")[0].reshape(64, 1))
    sh["dtb_bc"] = np.ascontiguousarray(np.broadcast_to(f("ssd_dt_bias")[0][None, :], (128, 64)))
    sh["alog_col"] = np.ascontiguousarray(f("ssd_a_log")[0].reshape(64, 1))
    sh["d_bc"] = np.ascontiguousarray(np.broadcast_to(f("ssd_d")[0][None, :], (128, 64)))
    sh["ssd_nw_col"] = _col(f("ssd_norm_w")[0], 32)
    sh["ssd_w_out"] = np.ascontiguousarray(f("ssd_w_out")[0])
    sh["moe_wr"] = np.ascontiguousarray(np.concatenate([f("moe_w_group"), f("moe_w_expert")], axis=-1))
    br = np.concatenate([f("moe_b_group"), f("moe_b_expert")], axis=-1)
    sh["moe_br_bc"] = np.ascontiguousarray(np.broadcast_to(br[:, None, :], (2, 128, 72)))
    sh["moe_wg"] = f("moe_w_gate")
    sh["moe_wu"] = f("moe_w_up")
    sh["moe_wd"] = f("moe_w_down")
    s_ = np.arange(128)[:, None]
    t_ = np.arange(128)[None, :]
    sh["c_ident"] = np.eye(128, dtype=np.float32)
    sh["c_tri"] = (s_ <= t_).astype(np.float32)
    sh["c_tris"] = (s_ < t_).astype(np.float32)
    sh["c_neg"] = np.where(s_ <= t_, 0.0, -30000.0).astype(np.float32)
    sh["c_ebase"] = np.ascontiguousarray(np.broadcast_to((np.arange(64) * CAP).astype(np.float32)[None, :], (128, 64)))
    rm = np.ones((64, 512), np.float32)
    rm[:, ::128] = 0.0
    sh["c_reset"] = rm
    return sh


def core_inputs(inp, b, sh=None):
    sh = sh if sh is not None else _shared_inputs(inp)
    m = dict(sh)
    m["xT"] = np.ascontiguousarray(np.asarray(inp["x"], np.float32)[b].T)
    return m


_NC_CACHE = {}


def kernel(**inputs):
    if "nc" not in _NC_CACHE:
        _NC_CACHE["nc"] = Builder(stop="all", dbg=False).build()
    nc = _NC_CACHE["nc"]
    sh = _shared_inputs(inputs)
    in_maps = [core_inputs(inputs, b, sh) for b in range(8)]
    res = run_bass_kernel_spmd(nc, in_maps, core_ids=list(range(8)))
    out = np.stack([np.ascontiguousarray(r["outT"].T) for r in res.results], axis=0)
    return out.astype(np.float32)
```
